# Optimizing a Trainium2 kernel written in Bass

```python
import math
import jax, jax.numpy as jnp
from jax import lax
import numpy as np

D_MODEL = 1024
BATCH = 8
SEQ = 4096
DEPTH = 4

CHUNK = 64
Q_BLOCK = 128
EPS = 1e-6
A_HEADS = 4
A_QK_DIM = 64
A_V_DIM = 2 * A_QK_DIM
A_WIDTH = A_HEADS * A_V_DIM
B_HEADS = 4
B_K_DIM = 64
B_V_DIM = 128
B_WIDTH = B_HEADS * B_V_DIM
B_GATE_RANK = 16
B_GATE_TAU = 16.0
BRANCH_WIDTH = A_WIDTH
N_BRANCH = 2
D_FF = 4 * D_MODEL
SPLITS = (A_HEADS * 2 * A_QK_DIM,
          A_HEADS * 2 * A_QK_DIM,
          A_WIDTH,
          B_HEADS * B_K_DIM,
          B_HEADS * B_K_DIM,
          B_WIDTH,
          B_GATE_RANK,
          B_WIDTH,
          N_BRANCH * D_MODEL)
D_IN = sum(SPLITS)

kernel_name = "hybrid_diffattn_gla_gated_merge"


def rms_norm(x, gain):
    xf = x.astype(jnp.float32)
    y = xf * lax.rsqrt(jnp.mean(xf * xf, axis=-1, keepdims=True) + EPS)
    return (y * gain.astype(jnp.float32)).astype(x.dtype)


def split_columns(proj):
    outs, start = [], 0
    for width in SPLITS:
        outs.append(proj[..., start:start + width])
        start += width
    return outs


def diff_attention(aq, ak, av, q_gain, k_gain, lq1, lk1, lq2, lk2, sub_gain, lambda_init):
    B, S = aq.shape[:2]
    q = rms_norm(aq.reshape(B, S, A_HEADS, 2, A_QK_DIM), q_gain)
    k = rms_norm(ak.reshape(B, S, A_HEADS, 2, A_QK_DIM), k_gain)
    v = av.reshape(B, S, A_HEADS, A_V_DIM)
    lam = (jnp.exp(jnp.sum(lq1.astype(jnp.float32) * lk1.astype(jnp.float32)))
           - jnp.exp(jnp.sum(lq2.astype(jnp.float32) * lk2.astype(jnp.float32)))
           + lambda_init)
    scale = A_QK_DIM ** -0.5
    n_blk = S // Q_BLOCK
    qb = q.reshape(B, n_blk, Q_BLOCK, A_HEADS, 2, A_QK_DIM).transpose(1, 0, 2, 3, 4, 5)
    key_chunk = jnp.arange(S) // CHUNK

    def block(args):
        qi, idx = args
        scores = jnp.einsum('bqhmd,bkhmd->bhmqk', qi, k).astype(jnp.float32) * scale
        q_chunk = (idx * Q_BLOCK + jnp.arange(Q_BLOCK)) // CHUNK
        mask = key_chunk[None, :] <= q_chunk[:, None]
        scores = jnp.where(mask, scores, -jnp.inf)
        p = jax.nn.softmax(scores, axis=-1)
        p = p[:, :, 0] - lam * p[:, :, 1]
        return jnp.einsum('bhqk,bkhe->bqhe', p.astype(v.dtype), v)

    out = lax.map(block, (qb, jnp.arange(n_blk)))
    out = out.transpose(1, 0, 2, 3, 4).reshape(B, S, A_HEADS, A_V_DIM)
    out = rms_norm(out, sub_gain) * (1.0 - lambda_init)
    return out.reshape(B, S, A_WIDTH)


def gla(bq, bk, bv, ba, br, w_gate_up, b_gate, out_gain):
    B, S = bq.shape[:2]
    nC = S // CHUNK
    f32 = jnp.float32
    z = (ba @ w_gate_up + b_gate).astype(f32)
    g = (jax.nn.log_sigmoid(z) / B_GATE_TAU).reshape(B, nC, CHUNK, B_HEADS, B_K_DIM)
    G = jnp.cumsum(g, axis=2)
    q = bq.reshape(B, nC, CHUNK, B_HEADS, B_K_DIM).astype(f32) * (B_K_DIM ** -0.5)
    k = bk.reshape(B, nC, CHUNK, B_HEADS, B_K_DIM).astype(f32)
    v = bv.reshape(B, nC, CHUNK, B_HEADS, B_V_DIM).astype(f32)
    q_dec = q * jnp.exp(G)
    k_dec = k * jnp.exp(-G)
    causal = jnp.tril(jnp.ones((CHUNK, CHUNK), dtype=bool))
    a = jnp.einsum('bnihd,bnjhd->bnhij', q_dec, k_dec)
    a = jnp.where(causal, a, 0.0)
    o_intra = jnp.einsum('bnhij,bnjhe->bnihe', a, v)
    G_last = G[:, :, -1]
    k_state = k * jnp.exp(G_last[:, :, None] - G)
    chunk_kv = jnp.einsum('bnjhd,bnjhe->bnhde', k_state, v)

    def step(state, inp):
        decay, kv = inp
        return decay[..., None] * state + kv, state

    init = jnp.zeros((B, B_HEADS, B_K_DIM, B_V_DIM), f32)
    _, states = lax.scan(step, init, (jnp.exp(G_last).transpose(1, 0, 2, 3),
                                      chunk_kv.transpose(1, 0, 2, 3, 4)))
    states = states.transpose(1, 0, 2, 3, 4)
    o_inter = jnp.einsum('bnihd,bnhde->bnihe', q_dec, states)
    o = (o_intra + o_inter).reshape(B, S, B_HEADS, B_V_DIM)
    r = br.reshape(B, S, B_HEADS, B_V_DIM).astype(f32)
    o = rms_norm(o, out_gain) * jax.nn.silu(r)
    return o.reshape(B, S, B_WIDTH).astype(bq.dtype)


def setup_inputs(seed: int = 0) -> dict:
    key = jax.random.key(seed)
    ks = jax.random.split(key, 18)
    L, D = DEPTH, D_MODEL

    def nrm(k, shape, scale):
        return jax.random.normal(k, shape, jnp.float32) * scale

    def gain(k, shape):
        return 1.0 + 0.02 * jax.random.normal(k, shape, jnp.float32)

    return {
        "x": nrm(ks[0], (BATCH, SEQ, D), 1.0),
        "norm_mix": gain(ks[1], (L, D)),
        "w_in": nrm(ks[2], (L, D, D_IN), D ** -0.5),
        "a_q_norm": gain(ks[3], (L, A_QK_DIM)),
        "a_k_norm": gain(ks[4], (L, A_QK_DIM)),
        "a_lambda_q1": nrm(ks[5], (L, A_QK_DIM), 0.1),
        "a_lambda_k1": nrm(ks[6], (L, A_QK_DIM), 0.1),
        "a_lambda_q2": nrm(ks[7], (L, A_QK_DIM), 0.1),
        "a_lambda_k2": nrm(ks[8], (L, A_QK_DIM), 0.1),
        "a_sub_norm": gain(ks[9], (L, A_V_DIM)),
        "b_gate_up": nrm(ks[10], (L, B_GATE_RANK, B_HEADS * B_K_DIM), B_GATE_RANK ** -0.5),
        "b_gate_bias": nrm(ks[11], (L, B_HEADS * B_K_DIM), 0.1),
        "b_out_norm": gain(ks[12], (L, B_V_DIM)),
        "w_branch": nrm(ks[13], (L, N_BRANCH, BRANCH_WIDTH, D), BRANCH_WIDTH ** -0.5),
        "w_out": nrm(ks[14], (L, D, D), D ** -0.5),
        "norm_ffn": gain(ks[15], (L, D)),
        "w_up": nrm(ks[16], (L, D, D_FF), D ** -0.5),
        "w_down": nrm(ks[17], (L, D_FF, D), 0.5 * D_FF ** -0.5),
    }


def reference(x, norm_mix, w_in, a_q_norm, a_k_norm, a_lambda_q1, a_lambda_k1,
              a_lambda_q2, a_lambda_k2, a_sub_norm, b_gate_up, b_gate_bias, b_out_norm,
              w_branch, w_out, norm_ffn, w_up, w_down):
    B, S, D = x.shape
    for l in range(DEPTH):
        lambda_init = 0.8 - 0.6 * math.exp(-0.3 * l)
        u = rms_norm(x, norm_mix[l])
        proj = u @ w_in[l]
        aq, ak, av, bq, bk, bv, ba, br, gates = split_columns(proj)
        ya = diff_attention(aq, ak, av, a_q_norm[l], a_k_norm[l], a_lambda_q1[l],
                            a_lambda_k1[l], a_lambda_q2[l], a_lambda_k2[l],
                            a_sub_norm[l], lambda_init)
        yb = gla(bq, bk, bv, ba, br, b_gate_up[l], b_gate_bias[l], b_out_norm[l])
        branches = jnp.stack([ya, yb], axis=2)
        branch_proj = jnp.einsum('bsnw,nwd->bsnd', branches, w_branch[l])
        gate = jax.nn.sigmoid(gates.reshape(B, S, N_BRANCH, D))
        mixed = jnp.sum(gate * branch_proj, axis=2) @ w_out[l]
        x = x + mixed
        h = rms_norm(x, norm_ffn[l])
        x = x + jnp.square(jax.nn.relu(h @ w_up[l])) @ w_down[l]
    return x
```

```python
import math
import numpy as np
import concourse.bass as bass
import concourse.mybir as mybir
from concourse.bass_utils import run_bass_kernel_spmd

F32 = mybir.dt.float32
BF16 = mybir.dt.bfloat16
AF = mybir.ActivationFunctionType
ALU = mybir.AluOpType

L_FULL, D, S_FULL, DIN, DFF = 4, 1024, 4096, 5136, 4096
TT = 512
EPS = 1e-6
NW = 3
NBLK = 33
NCOLS = 22
ENGS = ("pe", "act", "dve", "pool", "sp")


class Buf:
    __slots__ = ("name", "w", "r", "dsem", "dcnt")

    def __init__(self, name):
        self.name = name
        self.w = None
        self.r = {}
        self.dsem = None
        self.dcnt = 0


class Prog:
    def __init__(self, nc):
        self.nc = nc
        self.q = {e: [] for e in ENGS}
        self.sems = {}
        self.cnt = {e: 0 for e in ENGS}
        self.seen = {e: {} for e in ENGS}
        for e in ENGS:
            self.sems["E_" + e] = nc.alloc_semaphore(name="sem_" + e)
        self.ndsem = 0

    def _waits(self, eng, reads, writes):
        deps = {}
        for b in reads:
            if b.w is not None and deps.get(b.w[0], 0) < b.w[1]:
                deps[b.w[0]] = b.w[1]
        for b in writes:
            if b.w is not None and deps.get(b.w[0], 0) < b.w[1]:
                deps[b.w[0]] = b.w[1]
            for k, v in b.r.items():
                if deps.get(k, 0) < v:
                    deps[k] = v
        waits = []
        seen = self.seen[eng]
        for k, v in deps.items():
            if eng == "pe" and k == "E_pe":
                continue
            if seen.get(k, 0) >= v:
                continue
            seen[k] = v
            waits.append((k, v))
        return waits

    def _record(self, ev, reads, writes):
        k, v = ev
        for b in reads:
            if b.r.get(k, 0) < v:
                b.r[k] = v
        for b in writes:
            b.w = ev
            b.r = {}

    def op(self, eng, fn, reads=(), writes=()):
        waits = self._waits(eng, reads, writes)
        self.cnt[eng] += 1
        ev = ("E_" + eng, self.cnt[eng])
        self.q[eng].append((waits, fn, ("E_" + eng, 1)))
        self._record(ev, reads, writes)
        return ev

    def dma(self, eng, out, in_, home, reads=(), writes=()):
        if home.dsem is None:
            home.dsem = {}
            home.dcnt = {}
        if eng not in home.dsem:
            key = "D%d" % self.ndsem
            self.ndsem += 1
            self.sems[key] = self.nc.alloc_semaphore(name="dsem_%s" % key)
            home.dsem[eng] = key
            home.dcnt[eng] = 0
        waits = self._waits(eng, reads, writes)
        home.dcnt[eng] += 16
        ev = (home.dsem[eng], home.dcnt[eng])

        def fn(e, out=out, in_=in_):
            return e.dma_start(out=out, in_=in_)
        self.q[eng].append((waits, fn, (ev[0], 16)))
        self._record(ev, reads, writes)
        return ev

    def wait_all(self, eng, bufs):
        waits = self._waits(eng, bufs, ())
        self.q[eng].append((waits, None, None))

    def emit(self):
        nc = self.nc
        names = {"pe": "tensor", "act": "scalar", "dve": "vector", "pool": "gpsimd", "sp": "sync"}
        with nc.Block() as block:
            for e in ENGS:
                def body(eh, items=self.q[e], sems=self.sems):
                    for waits, fn, inc in items:
                        for k, v in waits:
                            eh.wait_ge(sems[k], v)
                        if fn is not None:
                            fn(eh).then_inc(sems[inc[0]], inc[1])
                getattr(block, names[e])(body)


def build(n_layers=L_FULL, n_tiles=S_FULL // TT, dbg_stop=False):
    S = n_tiles * TT
    nc = bass.Bass("TRN2", target_bir_lowering=False)
    x_in = nc.dram_tensor("x", [S, D], F32, kind="ExternalInput").ap()
    w_in = nc.dram_tensor("w_in", [L_FULL, D, DIN], F32, kind="ExternalInput").ap()
    w_br = nc.dram_tensor("w_branch", [L_FULL, 2, 512, D], F32, kind="ExternalInput").ap()
    w_out = nc.dram_tensor("w_out", [L_FULL, D, D], F32, kind="ExternalInput").ap()
    w_up = nc.dram_tensor("w_up", [L_FULL, D, DFF], F32, kind="ExternalInput").ap()
    w_dn = nc.dram_tensor("w_down", [L_FULL, DFF, D], F32, kind="ExternalInput").ap()
    wgu_in = nc.dram_tensor("wgu", [L_FULL, 16, 256], F32, kind="ExternalInput").ap()
    cols_in = nc.dram_tensor("cols", [128, L_FULL * NCOLS], F32, kind="ExternalInput").ap()
    lamv_in = nc.dram_tensor("lamv", [128, 4 * L_FULL * 64], F32, kind="ExternalInput").ap()
    cst_in = nc.dram_tensor("cst", [128, 1280], F32, kind="ExternalInput").ap()
    out = nc.dram_tensor("out", [S, D], F32, kind="ExternalOutput").ap()
    wsc = [nc.dram_tensor("wsc%d" % l, [NBLK, 128, 4096], BF16, kind="Internal").ap() for l in range(n_layers)]

    P = Prog(nc)
    def A(name, shape, dt):
        return nc.alloc_sbuf_tensor("s_" + name, shape, dt)
    kT = A("kT", [128, 4, S], BF16)
    vc = A("vc", [128, n_tiles * 4, 512], BF16)
    Sst = A("Sst", [128, 2, 2, 128], F32)
    xs = A("xs", [128, 2, 1024], F32)
    xt = A("xt", [128, 4, 1024], F32)
    uT = A("uT", [128, 8, 512], BF16)
    wbuf = A("wbuf", [128, NW, 4096], BF16)
    qT = A("qT", [128, 8, 512], BF16)
    PT = A("PT", [128, 4, 512], BF16)
    nt = A("nt", [128, 4, 512], F32)
    sqb = A("sqb", [128, 4, 512], BF16)
    ev = A("ev", [128, 2, 512], F32)
    sacc = A("sacc", [128, 2, 512], F32)
    yT = A("yT", [128, 8, 512], BF16)
    Rt = A("Rt", [128, 16384], BF16)
    cstf = A("cstf", [128, 1280], F32)
    cstb = A("cstb", [128, 640], BF16)
    cols = A("cols", [128, L_FULL * NCOLS], F32)
    dcol = A("dcol", [128, L_FULL * 8], F32)
    lamv = A("lamv", [128, 4 * L_FULL * 64], F32)
    lamt = A("lamt", [128, 4 * L_FULL], F32)
    wgu = A("wgu", [16, L_FULL, 256], BF16)
    st = A("st", [128, 16], F32)
    dec = A("dec", [128, 2, 8], F32)
    dbgz = A("dbgz", [128, 4, 512], F32) if dbg_stop else None
    ps = [nc.alloc_psum_tensor("ps%d" % i, [128, 512], F32) for i in range(8)]

    actT = Rt[:, :].rearrange("p (f t) -> p f t", t=512)
    fv = [Rt[:, i * 1024:(i + 1) * 1024].bitcast(F32) for i in range(5)]
    qdT = Rt[:, 5120:6144].rearrange("p (h t) -> p h t", t=512)
    kdT = Rt[:, 6144:7168].rearrange("p (h t) -> p h t", t=512)
    ks_t = Rt[:, 7168:8192].rearrange("p (h j c) -> p h j c", h=2, j=4)
    bv_t = Rt[:, 8192:10240].rearrange("p (j c) -> p j c", c=512)
    Sbf = Rt[:, 10240:12288].rearrange("p (h n e) -> p h n e", h=2, n=8)
    baT = Rt[:, 12288:12800]
    aTm = [Rt[:, 12800 + 512 * i:13312 + 512 * i].rearrange("p (j c) -> p j c", c=128) for i in range(4)]

    def mk(n, k=None):
        return Buf(n) if k is None else [Buf("%s%d" % (n, i)) for i in range(k)]
    kTb, vcb = mk("kT", n_tiles), mk("vc", n_tiles)
    Sb, xsb, xtb, uTb, wb = [mk("Sa", 2), mk("Sb", 2)], mk("xs", 2), mk("xt", 4), mk("uT", 4), mk("wb", NW)
    qTb, PTb, ntb, sqbb, yTb = mk("qT", 8), mk("PT", 4), mk("nt", 4), mk("sqb", 4), mk("yT", 8)
    mTb = PTb + sqbb
    mT_ap = [PT[:, i, :] for i in range(4)] + [sqb[:, i, :] for i in range(4)]
    evb, saccb = mk("ev", 2), mk("sacc", 2)
    actb, fvb = mk("act", 32), mk("fv", 5)
    qdb, kdb, kstb, sbfb, aTb = mk("qd", 2), mk("kd", 2), mk("kst", 2), mk("sbf", 2), mk("aT", 4)
    bvb, bab, decb, stb, gR = mk("bv"), mk("ba"), mk("dec", 2), mk("st"), mk("gR")
    cstB, psb = mk("cst"), mk("ps", 8)
    xd = [mk("xd%d_" % t, 4) for t in range(n_tiles)]
    cvb = [[Buf("cv%d_%d" % (l, g)) for g in range(8)] for l in range(n_layers)]

    ident = cstf[:, 0:128]
    rmask = cstf[:, 128:640]
    ones_b = cstb[:, 0:128]
    ones_f = cstf[:, 640:768]
    bd64_b = cstb[:, 128:256]
    o128_b = cstb[:, 256:384]
    mask2_b = cstb[:, 384:512]

    def blk_group(b):
        return min(7, b * 8 // NBLK)

    def conv_layer_ops(l):
        W = wsc[l]
        ops = []

        def kview(src2d):
            return src2d.rearrange("(k p) c -> p k c", p=128)

        def dst(b, k, c, c0=0, cw=None):
            v = W[b, :, 0:k * c].rearrange("p (k c) -> p k c", c=c)
            return v if cw is None else v[:, :, c0:c0 + cw]
        wi = w_in[l]
        ops.append((0, [(dst(0, 8, 512), kview(wi[:, 0:512]))]))
        ops.append((1, [(dst(1, 8, 512), kview(wi[:, 512:1024]))]))
        ops.append((2, [(dst(2, 8, 16), kview(wi[:, 2560:2576]))]))
        ops.append((3, [(dst(3, 8, 512), kview(wi[:, 1024:1536]))]))
        ops.append((4, [(dst(4, 8, 512), kview(wi[:, 1536:2048]))]))
        ops.append((5, [(dst(5, 8, 512), kview(wi[:, 2048:2560]))]))
        ops.append((6, [(dst(6, 8, 512), kview(wi[:, 2576:3088]))]))
        for i in range(4):
            g0 = 3088 + 2 * i * 128
            ops.append((7 + 2 * i, [(dst(7 + 2 * i, 8, 512, 0, 256), kview(wi[:, g0:g0 + 256])),
                                    (dst(7 + 2 * i, 8, 512, 256, 256), kview(wi[:, g0 + 1024:g0 + 1280]))]))
            b = 8 + 2 * i
            pairs = []
            for n in range(2):
                d = W[b, :, n * 1024:(n + 1) * 1024].rearrange("p (k c) -> p k c", c=256)
                pairs.append((d, kview(w_br[l, n][:, 2 * i * 128:2 * i * 128 + 256])))
            ops.append((b, pairs))
        for h in range(2):
            ops.append((15 + h, [(dst(15 + h, 8, 512), kview(w_out[l][:, h * 512:(h + 1) * 512]))]))
        for i in range(8):
            ops.append((17 + i, [(dst(17 + i, 8, 512), kview(w_up[l][:, i * 512:(i + 1) * 512]))]))
        for h in range(2):
            for fg in range(4):
                b = 25 + h * 4 + fg
                ops.append((b, [(dst(b, 8, 512), kview(w_dn[l][fg * 1024:(fg + 1) * 1024, h * 512:(h + 1) * 512]))]))
        return ops

    def emit_conv(l, groups):
        for b, pairs in conv_layer_ops(l):
            g = blk_group(b)
            if g in groups:
                for d, s_ in pairs:
                    P.dma("pool", d, s_, cvb[l][g], writes=[cvb[l][g]])

    P.dma("sp", cstf[:], cst_in, cstB, writes=[cstB])
    P.dma("sp", cols[:], cols_in, cstB, writes=[cstB])
    P.dma("sp", lamv[:], lamv_in, cstB, writes=[cstB])
    P.dma("pool", wgu[:], wgu_in.rearrange("l r c -> r l c"), cstB, writes=[cstB])
    emit_conv(0, set(range(8)))
    P.op("dve", lambda e: e.tensor_copy(out=cstb[:], in_=cstf[:, 640:1280]), reads=[cstB], writes=[cstB])
    P.op("dve", lambda e: e.memset(qT[:, :, :], 0.0), reads=[], writes=qTb)
    lv = lamv[:, :].rearrange("p (a l d) -> p a l d", a=4, l=L_FULL)
    P.op("dve", lambda e: e.tensor_tensor(out=lv[:, 0], in0=lv[:, 0], in1=lv[:, 1], op=ALU.mult),
         reads=[cstB], writes=[cstB])
    P.op("dve", lambda e: e.tensor_tensor(out=lv[:, 2], in0=lv[:, 2], in1=lv[:, 3], op=ALU.mult),
         reads=[cstB], writes=[cstB])
    P.op("dve", lambda e: e.tensor_reduce(out=lamt[:, 0:L_FULL], in_=lv[:, 0], axis=mybir.AxisListType.X,
                                          op=ALU.add), reads=[cstB], writes=[cstB])
    P.op("dve", lambda e: e.tensor_reduce(out=lamt[:, L_FULL:2 * L_FULL], in_=lv[:, 2], axis=mybir.AxisListType.X,
                                          op=ALU.add), reads=[cstB], writes=[cstB])
    P.op("act", lambda e: e.activation(out=lamt[:, 2 * L_FULL:4 * L_FULL], in_=lamt[:, 0:2 * L_FULL], func=AF.Exp),
         reads=[cstB], writes=[cstB])
    P.op("dve", lambda e: e.tensor_tensor(out=lamt[:, 0:L_FULL], in0=lamt[:, 3 * L_FULL:4 * L_FULL],
                                          in1=lamt[:, 2 * L_FULL:3 * L_FULL], op=ALU.subtract),
         reads=[cstB], writes=[cstB])
    for l in range(n_layers):
        li = 0.8 - 0.6 * math.exp(-0.3 * l)
        c = l * NCOLS
        dc_ = l * 8
        P.op("dve", lambda e, l=l, li=li, dc_=dc_: e.tensor_scalar(
            out=dcol[:, dc_:dc_ + 1], in0=lamt[:, l:l + 1], scalar1=-li, scalar2=None, op0=ALU.add),
            reads=[cstB], writes=[cstB])
        P.op("dve", lambda e, c=c, dc_=dc_: e.tensor_scalar(
            out=dcol[:, dc_ + 1:dc_ + 2], in0=cols[:, c + 20:c + 21], scalar1=0.125, scalar2=None, op0=ALU.mult),
            reads=[cstB], writes=[cstB])
        P.op("dve", lambda e, c=c, dc_=dc_, li=li: e.tensor_scalar(
            out=dcol[:, dc_ + 2:dc_ + 3], in0=cols[:, c + 18:c + 19], scalar1=1.0 - li, scalar2=None, op0=ALU.mult),
            reads=[cstB], writes=[cstB])
        P.op("dve", lambda e, c=c, dc_=dc_: e.tensor_scalar(
            out=dcol[:, dc_ + 3:dc_ + 5], in0=cols[:, c + 16:c + 18], scalar1=-1.0, scalar2=None, op0=ALU.mult),
            reads=[cstB], writes=[cstB])

    state = {"wslot": 0, "gen": 0, "wide": 0, "xsl": 0, "stc": 0}

    def load_block(l, b, nelem=4096):
        s = state["wslot"]
        state["wslot"] = (s + 1) % NW
        P.dma("sp", wbuf[:, s, 0:nelem], wsc[l][b, :, 0:nelem], wb[s],
              reads=[cvb[l][blk_group(b)]], writes=[wb[s]])
        return s

    def genbank():
        i = state["gen"]
        state["gen"] = (i + 1) % 4
        return i

    def widebank():
        i = state["wide"]
        state["wide"] = (i + 1) % 8
        return i

    def w3(s, c=512, k=8):
        return wbuf[:, s, 0:k * c].rearrange("p (k c) -> p k c", c=c)

    def mm_group(outs, reads, writes):
        def fn(e):
            ins = None
            for o, pairs in outs:
                n = len(pairs)
                for i, (a, b) in enumerate(pairs):
                    ins = e.matmul(o, lhsT=a, rhs=b, start=(i == 0), stop=(i == n - 1))
            return ins
        return P.op("pe", fn, reads=reads, writes=writes)

    def rstd_from_ms(ms_ap, out_ap, reads, writes, tmpbuf, tmp_ap):
        P.op("act", lambda e: e.activation(out=tmp_ap, in_=ms_ap, func=AF.Ln, bias=EPS_AP),
             reads=reads + [cstB], writes=[tmpbuf])
        P.op("act", lambda e: e.activation(out=out_ap, in_=tmp_ap, func=AF.Exp, scale=-0.5),
             reads=[tmpbuf], writes=writes)

    epsc = dcol[:, L_FULL * 8 - 1:L_FULL * 8]
    P.op("dve", lambda e: e.memset(epsc, EPS), reads=[], writes=[cstB])
    EPS_AP = epsc

    def norm_T(l, tt, which, src_dram):
        for j in range(4):
            norm_sub(l, tt, which, src_dram, j)

    def norm_sub(l, tt, which, src_dram, j):
        norm_B(*norm_A(l, tt, which, src_dram, j))

    def norm_A(l, tt, which, src_dram, j):
        if True:
            sl = state["xsl"]
            state["xsl"] = 1 - sl
            sc = state["stc"]
            state["stc"] = (sc + 1) % 4
            c = sc * 4
            if which == 0:
                r0 = tt * TT + j * 128
                P.dma("pool", xs[:, sl, :], src_dram[r0:r0 + 128, :], xsb[sl],
                      reads=[xd[tt][j]], writes=[xsb[sl]])
                src, srcb = xs[:, sl, :], xsb[sl]
            else:
                src, srcb = xt[:, j, :], xtb[j]
            jk = ev[:, :, :].rearrange("p a b -> p (a b)")
            P.op("act", lambda e, src=src, c=c: e.activation(out=jk, in_=src, func=AF.Square,
                                                               accum_out=st[:, c:c + 1]),
                 reads=[srcb], writes=[evb[0], evb[1], stb])
            P.op("act", lambda e, c=c: e.activation(out=st[:, c + 1:c + 2], in_=st[:, c:c + 1], func=AF.Ln,
                                                    scale=1.0 / D, bias=EPS_AP), reads=[stb, cstB], writes=[stb])
            P.op("act", lambda e, c=c: e.activation(out=st[:, c + 2:c + 3], in_=st[:, c + 1:c + 2], func=AF.Exp,
                                                    scale=-0.5), reads=[stb], writes=[stb])
            P.op("dve", lambda e, src=src, sl=sl, c=c: e.tensor_scalar(
                out=xs[:, sl, :], in0=src, scalar1=st[:, c + 2:c + 3], scalar2=None, op0=ALU.mult),
                reads=[srcb, stb], writes=[xsb[sl]])
        return (l, which, j, sl)

    def norm_B(l, which, j, sl):
        gc0 = l * NCOLS + (0 if which == 0 else 8)
        if True:
            for half in range(2):
                bk = genbank()

                def tr(e, sl=sl, half=half, bk=bk):
                    ins = None
                    for k in range(4):
                        kc = half * 4 + k
                        ins = e.transpose(out=ps[bk][:, k * 128:(k + 1) * 128],
                                          in_=xs[:, sl, kc * 128:(kc + 1) * 128], identity=ident)
                    return ins
                P.op("pe", tr, reads=[xsb[sl], cstB], writes=[psb[bk]])
                g = cols[:, gc0 + half * 4:gc0 + half * 4 + 4].unsqueeze(2).to_broadcast([128, 4, 128])
                P.op("dve", lambda e, half=half, bk=bk, j=j, g=g: e.tensor_tensor(
                    out=uT[:, half * 4:half * 4 + 4, j * 128:(j + 1) * 128],
                    in0=ps[bk][:, :].rearrange("p (k t) -> p k t", t=128), in1=g, op=ALU.mult),
                    reads=[psb[bk], cstB], writes=[uTb[j]])

    def fm_chunk(s, c0, bk, extra_reads=()):
        w = w3(s)
        mm_group([(ps[bk][:, :], [(w[:, kc, c0:c0 + 128], uT[:, kc, :]) for kc in range(8)])],
                 reads=[wb[s]] + uTb + list(extra_reads), writes=[psb[bk]])

    def tile(l, tt):
        src_dram = x_in if l == 0 else out
        C = l * NCOLS
        DC = l * 8
        s = load_block(l, 2, 128)
        bk = genbank()
        wv = w3(s, 16, 8)
        mm_group([(ps[bk][0:16, :], [(wv[:, kc, :], uT[:, kc, :]) for kc in range(8)])],
                 reads=[wb[s]] + uTb, writes=[psb[bk]])
        P.op("dve", lambda e, bk=bk: e.tensor_copy(out=baT[0:16, :], in_=ps[bk][0:16, :]),
             reads=[psb[bk]], writes=[bab, gR])
        s = load_block(l, 3)
        w = w3(s)
        for j in range(4):
            bk = genbank()
            mm_group([(ps[bk][:, :], [(uT[:, kc, j * 128:(j + 1) * 128], w[:, kc, :]) for kc in range(8)])],
                     reads=[wb[s]] + uTb, writes=[psb[bk]])
            P.op("act", lambda e, bk=bk, j=j: e.activation(out=vc[:, tt * 4 + j, :], in_=ps[bk][:, :], func=AF.Copy),
                 reads=[psb[bk]], writes=[vcb[tt]])
        s_qk = load_block(l, 4)
        s_bv = load_block(l, 5)
        wbv = w3(s_bv)
        for hc in range(2):
            bk = genbank()
            mm_group([(ps[bk][:, :], [(wgu[:, l, hc * 128:(hc + 1) * 128], baT[0:16, :])])],
                     reads=[bab, cstB, gR], writes=[psb[bk]])
            nb = dcol[:, DC + 3 + hc:DC + 4 + hc]
            P.op("act", lambda e, bk=bk, nb=nb: e.activation(out=fv[0], in_=ps[bk][:, :], func=AF.Exp,
                                                             scale=-1.0, bias=nb),
                 reads=[psb[bk], cstB, gR], writes=[fvb[0]])
            P.op("act", lambda e: e.activation(out=fv[0], in_=fv[0], func=AF.Ln, bias=ONE_AP),
                 reads=[fvb[0], cstB, gR], writes=[fvb[0]])
            P.op("dve", lambda e: e.tensor_tensor_scan(out=fv[1], data0=rmask, data1=fv[0], initial=0.0,
                                                       op0=ALU.mult, op1=ALU.add),
                 reads=[fvb[0], cstB, gR], writes=[fvb[1]])
            P.op("act", lambda e: e.activation(out=fv[2], in_=fv[1], func=AF.Exp, scale=-1.0 / 16),
                 reads=[fvb[1], gR], writes=[fvb[2]])
            P.op("act", lambda e: e.activation(out=fv[0], in_=fv[1], func=AF.Exp, scale=1.0 / 16),
                 reads=[fvb[1], gR], writes=[fvb[0]])
            csv = fv[1].rearrange("p (n t) -> p n t", t=64)
            P.op("dve", lambda e, csv=csv: e.tensor_tensor(
                out=fv[3].rearrange("p (n t) -> p n t", t=64), in0=csv[:, :, 63:64].to_broadcast([128, 8, 64]),
                in1=csv, op=ALU.subtract), reads=[fvb[1], gR], writes=[fvb[3]])
            P.op("act", lambda e: e.activation(out=fv[3], in_=fv[3], func=AF.Exp, scale=-1.0 / 16),
                 reads=[fvb[3], gR], writes=[fvb[3]])
            egv = fv[2].rearrange("p (n t) -> p n t", t=64)
            P.op("dve", lambda e, hc=hc, egv=egv: e.tensor_copy(out=dec[:, hc, :], in_=egv[:, :, 63]),
                 reads=[fvb[2], gR], writes=[decb[hc]])
            for j in (2 * hc, 2 * hc + 1):
                bkv = genbank()
                mm_group([(ps[bkv][:, :], [(uT[:, kc, j * 128:(j + 1) * 128], wbv[:, kc, :]) for kc in range(8)])],
                         reads=[wb[s_bv]] + uTb, writes=[psb[bkv]])
                P.op("act", lambda e, bkv=bkv, j=j: e.activation(out=bv_t[:, j, :], in_=ps[bkv][:, :], func=AF.Copy),
                     reads=[psb[bkv], gR], writes=[bvb])
            bq = genbank()
            fm_chunk(s_qk, hc * 128, bq)
            P.op("dve", lambda e, bq=bq, hc=hc: e.scalar_tensor_tensor(
                out=qdT[:, hc, :], in0=ps[bq][:, :], scalar=0.125, in1=fv[2], op0=ALU.mult, op1=ALU.mult),
                reads=[psb[bq], fvb[2], gR], writes=[qdb[hc]])
            bkk = genbank()
            fm_chunk(s_qk, 256 + hc * 128, bkk)
            P.op("dve", lambda e, bkk=bkk, hc=hc: e.tensor_tensor(
                out=kdT[:, hc, :], in0=ps[bkk][:, :], in1=fv[0], op=ALU.mult),
                reads=[psb[bkk], fvb[0], gR], writes=[kdb[hc]])
            P.op("dve", lambda e, bkk=bkk: e.tensor_tensor(
                out=fv[4], in0=ps[bkk][:, :], in1=fv[3], op=ALU.mult),
                reads=[psb[bkk], fvb[3], gR], writes=[fvb[4]])
            bt = genbank()

            def trk(e, bt=bt):
                ins = None
                for j in range(4):
                    ins = e.transpose(out=ps[bt][:, j * 128:(j + 1) * 128], in_=fv[4][:, j * 128:(j + 1) * 128],
                                      identity=ident)
                return ins
            P.op("pe", trk, reads=[fvb[4], cstB, gR], writes=[psb[bt]])
            P.op("act", lambda e, bt=bt, hc=hc: e.activation(
                out=ks_t[:, hc, :, :], in_=ps[bt][:, :].rearrange("p (j c) -> p j c", c=128), func=AF.Copy),
                reads=[psb[bt], gR], writes=[kstb[hc]])
        if tt == 0:
            P.op("dve", lambda e: e.memset(Sst[:, 0, :, :], 0.0), reads=[], writes=[Sb[0][0], Sb[0][1]])
        for j in range(4):
            kb = [genbank(), genbank()]
            for half in range(2):
                pr = slice(half * 64, (half + 1) * 64)
                outs = []
                for h in range(4):
                    outs.append((ps[kb[half]][:, h * 128:(h + 1) * 128],
                                 [(ks_t[pr, h // 2, j, :], bv_t[pr, j, h * 128:(h + 1) * 128])]))
                mm_group(outs, reads=[kstb[0], kstb[1], bvb, gR], writes=[psb[kb[half]]])
            for half in range(2):
                n = 2 * j + half
                pi, po = n % 2, (n + 1) % 2
                for hc in range(2):
                    P.op("act", lambda e, hc=hc, n=n, pi=pi: e.activation(out=Sbf[:, hc, n, :], in_=Sst[:, pi, hc, :],
                                                                          func=AF.Copy),
                         reads=[Sb[pi][hc], gR], writes=[sbfb[hc]])
                    for hh in range(2):
                        h = 2 * hc + hh
                        pr = slice(hh * 64, (hh + 1) * 64)
                        P.op("dve", lambda e, hc=hc, n=n, h=h, pr=pr, half=half, kb=kb, pi=pi, po=po:
                             e.scalar_tensor_tensor(
                            out=Sst[pr, po, hc, :], in0=Sst[pr, pi, hc, :], scalar=dec[pr, hc, n:n + 1],
                            in1=ps[kb[half]][pr, h * 128:(h + 1) * 128], op0=ALU.mult, op1=ALU.add),
                            reads=[Sb[pi][hc], decb[hc], psb[kb[half]]], writes=[Sb[po][hc]])
        for h in range(4):
            hc, hh = h // 2, h % 2
            pr = slice(hh * 64, (hh + 1) * 64)
            ba_ = genbank()
            mm_group([(ps[ba_][:, j * 128:(j + 1) * 128],
                       [(kdT[pr, hc, j * 128:(j + 1) * 128], qdT[pr, hc, j * 128:(j + 1) * 128])]) for j in range(4)],
                     reads=[kdb[hc], qdb[hc], gR], writes=[psb[ba_]])
            P.op("dve", lambda e, ba_=ba_, h=h: e.tensor_tensor(
                out=aTm[h], in0=ps[ba_][:, :].rearrange("p (j c) -> p j c", c=128),
                in1=mask2_b.unsqueeze(1).to_broadcast([128, 4, 128]), op=ALU.mult),
                reads=[psb[ba_], cstB, gR], writes=[aTb[h]])
        sblk = [load_block(l, 0), load_block(l, 1)]
        prev = None

        def qk_tail(c, bk):
            isk, h = c // 4, c % 4
            gcol = dcol[:, DC + 1:DC + 2] if isk == 0 else cols[:, C + 21:C + 22]
            sq = sqb[:, c % 2, :]
            b2 = widebank()
            mm_group([(ps[b2][:, :], [(bd64_b, sq)])], reads=[sqbb[c % 2], cstB], writes=[psb[b2]])
            ti = c % 2
            rstd_from_ms(ps[b2][:, :], nt[:, ti, :], [psb[b2]], [ntb[ti]], ntb[2 + ti], nt[:, 2 + ti, :])
            if isk == 0:
                for m in range(2):
                    pr = slice(m * 64, (m + 1) * 64)
                    P.op("dve", lambda e, m=m, pr=pr: e.scalar_tensor_tensor(
                        out=qT[pr, 2 * h + m, :], in0=ps[bk][pr, :], scalar=gcol[pr, :], in1=nt[pr, ti, :],
                        op0=ALU.mult, op1=ALU.mult),
                        reads=[psb[bk], ntb[ti], cstB], writes=[qTb[2 * h + m]])
            else:
                o_ap, o_b = kT[:, h, tt * TT:(tt + 1) * TT], [kTb[tt]]
                P.op("dve", lambda e: e.scalar_tensor_tensor(
                    out=o_ap, in0=ps[bk][:, :], scalar=gcol, in1=nt[:, ti, :], op0=ALU.mult, op1=ALU.mult),
                    reads=[psb[bk], ntb[ti], cstB], writes=o_b)
        for c in range(8):
            bk = widebank()
            fm_chunk(sblk[c // 4], (c % 4) * 128, bk)
            sq = sqb[:, c % 2, :]
            P.op("act", lambda e, bk=bk, sq=sq: e.activation(out=sq, in_=ps[bk][:, :], func=AF.Square),
                 reads=[psb[bk]], writes=[sqbb[c % 2]])
            if prev is not None:
                qk_tail(*prev)
            prev = (c, bk)
        qk_tail(*prev)
        nk = 4 * (tt + 1)

        if True:
            def score(h, m, kt):
                bk = genbank()
                a = kt - 4 * tt
                c0 = a * 128 if a > 0 else 0
                mm_group([(ps[bk][:, c0:512], [(kT[:, h, kt * 128:(kt + 1) * 128], qT[:, 2 * h + m, c0:512])])],
                         reads=[kTb[kt // 4], qTb[2 * h + m]], writes=[psb[bk]])
                sl = bk
                P.op("act", lambda e: e.activation(out=PT[:, sl, c0:512], in_=ps[bk][:, c0:512], func=AF.Exp),
                     reads=[psb[bk]], writes=[PTb[sl]])
                if a >= 0:
                    P.op("dve", lambda e: e.memset(PT[64:128, sl, c0:c0 + 64], 0.0), reads=[], writes=[PTb[sl]])
                return (h, m, kt, sl, c0)

            def pv(h, m, kt, sl, c0):
                first, last = (kt == 0), (kt == nk - 1)
                o1 = ps[4 + m][:, c0:512]
                vv, pp = vc[:, kt, h * 128:(h + 1) * 128], PT[:, sl, c0:512]
                P.op("pe", lambda e: e.matmul(o1, lhsT=vv, rhs=pp, start=first, stop=last),
                     reads=[vcb[kt // 4], PTb[sl]], writes=[psb[4 + m]])
                if first:
                    P.op("dve", lambda e: e.tensor_copy(out=sacc[:, m, :], in_=pp), reads=[PTb[sl]], writes=[saccb[m]])
                else:
                    P.op("dve", lambda e: e.tensor_tensor(out=sacc[:, m, c0:512], in0=sacc[:, m, c0:512], in1=pp,
                                                          op=ALU.add), reads=[PTb[sl], saccb[m]], writes=[saccb[m]])

        def attn_evac(h):
            for m in range(2):
                mm_group([(ps[6 + m][:, :], [(ones_f, sacc[:, m, :])])], reads=[saccb[m], cstB], writes=[psb[6 + m]])
            P.op("dve", lambda e: e.tensor_copy(out=ev[:, 0, :], in_=ps[4][:, :]), reads=[psb[4]], writes=[evb[0]])
            P.op("dve", lambda e: e.tensor_copy(out=ev[:, 1, :], in_=ps[5][:, :]), reads=[psb[5]], writes=[evb[1]])
            for m in range(2):
                P.op("act", lambda e, m=m: e.activation(out=nt[:, 1 + 2 * m, :], in_=ps[6 + m][:, :], func=AF.Ln),
                     reads=[psb[6 + m]], writes=[ntb[1 + 2 * m]])
                P.op("act", lambda e, m=m: e.activation(out=nt[:, 1 + 2 * m, :], in_=nt[:, 1 + 2 * m, :], func=AF.Exp,
                                                        scale=-1.0), reads=[ntb[1 + 2 * m]], writes=[ntb[1 + 2 * m]])
                P.op("dve", lambda e, m=m: e.tensor_tensor(out=ev[:, m, :], in0=ev[:, m, :], in1=nt[:, 1 + 2 * m, :],
                                                           op=ALU.mult), reads=[evb[m], ntb[1 + 2 * m]], writes=[evb[m]])
            P.op("dve", lambda e: e.scalar_tensor_tensor(out=ev[:, 0, :], in0=ev[:, 1, :], scalar=dcol[:, DC:DC + 1],
                                                         in1=ev[:, 0, :], op0=ALU.mult, op1=ALU.add),
                 reads=[evb[1], evb[0], cstB], writes=[evb[0]])
            P.op("act", lambda e: e.activation(out=sqb[:, 2, :], in_=ev[:, 0, :], func=AF.Square),
                 reads=[evb[0]], writes=[sqbb[2]])

        def attn_tail(h):
            b2 = genbank()
            mm_group([(ps[b2][:, :], [(o128_b, sqb[:, 2, :])])], reads=[sqbb[2], cstB], writes=[psb[b2]])
            rstd_from_ms(ps[b2][:, :], nt[:, 0, :], [psb[b2]], [ntb[0]], ntb[2], nt[:, 2, :])
            P.op("dve", lambda e: e.scalar_tensor_tensor(
                out=yT[:, h, :], in0=ev[:, 0, :], scalar=dcol[:, DC + 2:DC + 3], in1=nt[:, 0, :],
                op0=ALU.mult, op1=ALU.mult), reads=[evb[0], ntb[0], cstB], writes=[yTb[h]])
        items = [(h, m, kt) for h in range(4) for kt in range(nk) for m in range(2)]
        pend = []

        def retire():
            d = pend.pop(0)
            pv(*d)
            if d[1] == 1 and d[2] == nk - 1:
                if d[0] > 0:
                    attn_tail(d[0] - 1)
                attn_evac(d[0])
        for it in items:
            pend.append(score(*it))
            if len(pend) > 2:
                retire()
        while pend:
            retire()
        s_br = load_block(l, 6)
        for h in range(4):
            hc, hh = h // 2, h % 2
            pr = slice(hh * 64, (hh + 1) * 64)
            ob = 4 + h

            def ofn(e, h=h, hc=hc, pr=pr, ob=ob):
                ins = None
                for j in range(4):
                    e.matmul(ps[ob][:, j * 128:(j + 1) * 128], lhsT=bv_t[:, j, h * 128:(h + 1) * 128],
                             rhs=aTm[h][:, j, :], start=True, stop=False)
                    for half in range(2):
                        n = 2 * j + half
                        ins = e.matmul(ps[ob][:, n * 64:(n + 1) * 64], lhsT=Sbf[pr, hc, n, :],
                                       rhs=qdT[pr, hc, n * 64:(n + 1) * 64], start=False, stop=(half == 1))
                return ins
            P.op("pe", ofn, reads=[bvb, aTb[h], sbfb[hc], qdb[hc], gR], writes=[psb[ob]])
            if h == 0:
                attn_tail(3)
            P.op("act", lambda e, ob=ob, h=h: e.activation(out=sqb[:, h, :], in_=ps[ob][:, :], func=AF.Square),
                 reads=[psb[ob]], writes=[sqbb[h]])
        for hp in range(2):
            gb = {}
            for h in (2 * hp, 2 * hp + 1):
                b2 = genbank()
                mm_group([(ps[b2][:, :], [(o128_b, sqb[:, h, :])])], reads=[sqbb[h], cstB], writes=[psb[b2]])
                b3 = genbank()
                fm_chunk(s_br, h * 128, b3)
                gb[h] = (b2, b3)
            for h in (2 * hp, 2 * hp + 1):
                b2, b3 = gb[h]
                ob = 4 + h
                rstd_from_ms(ps[b2][:, :], nt[:, 0, :], [psb[b2]], [ntb[0]], ntb[2], nt[:, 2, :])
                P.op("act", lambda e, b3=b3: e.activation(out=nt[:, 3, :], in_=ps[b3][:, :], func=AF.Silu),
                     reads=[psb[b3]], writes=[ntb[3]])
                P.op("dve", lambda e, ob=ob: e.scalar_tensor_tensor(
                    out=nt[:, 1, :], in0=ps[ob][:, :], scalar=cols[:, C + 19:C + 20], in1=nt[:, 0, :],
                    op0=ALU.mult, op1=ALU.mult), reads=[psb[ob], ntb[0], cstB], writes=[ntb[1]])
                P.op("dve", lambda e, h=h: e.tensor_tensor(out=yT[:, 4 + h, :], in0=nt[:, 1, :], in1=nt[:, 3, :],
                                                           op=ALU.mult), reads=[ntb[1], ntb[3]], writes=[yTb[4 + h]])
        for j in range(4):
            r0 = tt * TT + j * 128
            P.dma("pool", xt[:, j, :], src_dram[r0:r0 + 128, :], xtb[j], reads=[xd[tt][j]], writes=[xtb[j]])
        for i in range(4):
            sg = load_block(l, 7 + 2 * i)
            sb_ = load_block(l, 8 + 2 * i, 2048)
            wbr = wbuf[:, sb_, 0:2048].rearrange("p (n k c) -> p n k c", n=2, k=4)
            for dd in range(2):
                dcn = 2 * i + dd
                gb = []
                for n in range(2):
                    bk = genbank()
                    fm_chunk(sg, n * 256 + dd * 128, bk)
                    P.op("act", lambda e, bk=bk, n=n: e.activation(out=nt[:, n, :], in_=ps[bk][:, :], func=AF.Sigmoid),
                         reads=[psb[bk]], writes=[ntb[n]])
                for n in range(2):
                    bk = genbank()
                    gb.append(bk)
                    mm_group([(ps[bk][:, :], [(wbr[:, n, kc, dd * 128:(dd + 1) * 128], yT[:, 4 * n + kc, :])
                                              for kc in range(4)])],
                             reads=[wb[sb_]] + yTb[4 * n:4 * n + 4], writes=[psb[bk]])
                P.op("dve", lambda e, gb=gb: e.tensor_tensor(out=nt[:, 2, :], in0=ps[gb[0]][:, :], in1=nt[:, 0, :],
                                                             op=ALU.mult), reads=[psb[gb[0]], ntb[0]], writes=[ntb[2]])
                P.op("dve", lambda e, gb=gb: e.tensor_tensor(out=nt[:, 3, :], in0=ps[gb[1]][:, :], in1=nt[:, 1, :],
                                                             op=ALU.mult), reads=[psb[gb[1]], ntb[1]], writes=[ntb[3]])
                P.op("dve", lambda e, dcn=dcn: e.tensor_tensor(out=mT_ap[dcn], in0=nt[:, 2, :], in1=nt[:, 3, :],
                                                               op=ALU.add), reads=[ntb[2], ntb[3]], writes=[mTb[dcn]])
        so = [load_block(l, 15), load_block(l, 16)]
        if dbg_stop:
            return

        def outp(j):
            for half in range(2):
                w = w3(so[half])
                bk = genbank()
                mm_group([(ps[bk][:, :], [(mT_ap[kc][:, j * 128:(j + 1) * 128], w[:, kc, :]) for kc in range(8)])],
                         reads=[wb[so[half]]] + mTb, writes=[psb[bk]])
                P.op("dve", lambda e, bk=bk, half=half: e.tensor_tensor(
                    out=xt[:, j, half * 512:(half + 1) * 512], in0=ps[bk][:, :],
                    in1=xt[:, j, half * 512:(half + 1) * 512], op=ALU.add),
                    reads=[psb[bk], xtb[j]], writes=[xtb[j]])
        outp(0)
        outp(1)
        norm_sub(l, tt, 1, None, 0)
        outp(2)
        norm_sub(l, tt, 1, None, 1)
        outp(3)
        norm_sub(l, tt, 1, None, 2)
        norm_sub(l, tt, 1, None, 3)
        for i in range(8):
            s = load_block(l, 17 + i)
            for f in range(4):
                fc = 4 * i + f
                bk = genbank()
                fm_chunk(s, f * 128, bk)
                ti = fc % 2
                P.op("act", lambda e, bk=bk, ti=ti: e.activation(out=nt[:, ti, :], in_=ps[bk][:, :], func=AF.Relu),
                     reads=[psb[bk]], writes=[ntb[ti]])
                wr = [actb[fc]] + ([gR] if fc == 0 else [])
                rd = [ntb[ti]] + ([] if fc == 0 else [gR])
                P.op("dve", lambda e, ti=ti, fc=fc: e.tensor_tensor(out=actT[:, fc, :], in0=nt[:, ti, :],
                                                                    in1=nt[:, ti, :], op=ALU.mult),
                     reads=rd, writes=wr)
        nxt = (l, tt + 1) if tt + 1 < n_tiles else ((l + 1, 0) if l + 1 < n_layers else None)
        nsrc = None if nxt is None else (x_in if nxt[0] == 0 else out)
        hA = []
        if nxt is not None:
            hA = [norm_A(nxt[0], nxt[1], 0, nsrc, 0), norm_A(nxt[0], nxt[1], 0, nsrc, 1)]
        for half in range(2):
            for fg in range(4):
                if nxt is not None and half == 0 and fg >= 1:
                    norm_B(*hA.pop(0))
                    if fg + 1 < 4:
                        hA.append(norm_A(nxt[0], nxt[1], 0, nsrc, fg + 1))
                if nxt is not None and half == 1 and fg == 0:
                    norm_B(*hA.pop(0))
                s = load_block(l, 25 + half * 4 + fg)
                w = w3(s)

                def dfn(e, fg=fg, w=w):
                    ins = None
                    for j in range(4):
                        for f in range(8):
                            fc = fg * 8 + f
                            ins = e.matmul(ps[4 + j][:, :], lhsT=actT[:, fc, j * 128:(j + 1) * 128], rhs=w[:, f, :],
                                           start=(fc == 0), stop=(fc == 31))
                    return ins
                P.op("pe", dfn, reads=[wb[s], gR] + actb[fg * 8:fg * 8 + 8], writes=psb[4:8])
            for j in range(4):
                P.op("dve", lambda e, j=j, half=half: e.tensor_tensor(
                    out=xt[:, j, half * 512:(half + 1) * 512], in0=ps[4 + j][:, :],
                    in1=xt[:, j, half * 512:(half + 1) * 512], op=ALU.add),
                    reads=[psb[4 + j], xtb[j]], writes=[xtb[j]])
        for j in range(4):
            r0 = tt * TT + j * 128
            P.dma("pool", out[r0:r0 + 128, :], xt[:, j, :], xtb[j], reads=[xtb[j]], writes=[xd[tt][j]])
        if l + 1 < n_layers:
            emit_conv(l + 1, {tt} if n_tiles == 8 else set(range(8)) if tt == 0 else set())

    onec = dcol[:, L_FULL * 8 - 2:L_FULL * 8 - 1]
    P.op("dve", lambda e: e.memset(onec, 1.0), reads=[], writes=[cstB])
    ONE_AP = onec

    norm_T(0, 0, 0, x_in)
    for l in range(n_layers):
        for tt in range(n_tiles):
            tile(l, tt)
    P.wait_all("pool", [b for t in xd for b in t])
    P.emit()
    return nc


def host_consts():
    c = np.zeros((128, 1280), np.float32)
    c[:, 0:128] = np.eye(128, dtype=np.float32)
    rm = np.ones((128, 512), np.float32)
    rm[:, ::64] = 0.0
    c[:, 128:640] = rm
    c[:, 640:768] = 1.0
    bd = np.zeros((128, 128), np.float32)
    bd[:64, :64] = 1.0 / 64
    bd[64:, 64:] = 1.0 / 64
    c[:, 768:896] = bd
    c[:, 896:1024] = 1.0 / 128
    jj, ii = np.meshgrid(np.arange(128), np.arange(128), indexing="ij")
    m2 = ((ii >= jj) & ((ii // 64) == (jj // 64))).astype(np.float32)
    c[:, 1024:1152] = m2
    return c


def layout_params(inp, n_layers=L_FULL):
    cols = np.zeros((128, L_FULL * NCOLS), np.float32)
    for l in range(L_FULL):
        c = l * NCOLS
        cols[:, c:c + 8] = inp["norm_mix"][l].reshape(8, 128).T
        cols[:, c + 8:c + 16] = inp["norm_ffn"][l].reshape(8, 128).T
        cols[:, c + 16:c + 18] = inp["b_gate_bias"][l].reshape(2, 128).T
        cols[:, c + 18] = inp["a_sub_norm"][l]
        cols[:, c + 19] = inp["b_out_norm"][l]
        cols[:, c + 20] = np.concatenate([inp["a_q_norm"][l], inp["a_q_norm"][l]])
        cols[:, c + 21] = np.concatenate([inp["a_k_norm"][l], inp["a_k_norm"][l]])
    lam = np.stack([inp["a_lambda_q1"], inp["a_lambda_k1"], inp["a_lambda_q2"], inp["a_lambda_k2"]], 0)
    lamv = np.ascontiguousarray(np.broadcast_to(lam.reshape(1, -1), (128, 4 * L_FULL * 64))).astype(np.float32)
    return cols, lamv


_NC_CACHE = {}


def kernel(**inputs):
    inp = {k: np.asarray(v) for k, v in inputs.items()}
    x = np.ascontiguousarray(inp["x"], dtype=np.float32)
    B = x.shape[0]
    cols, lamv = layout_params(inp)
    cst = host_consts()
    if "nc" not in _NC_CACHE:
        _NC_CACHE["nc"] = build()
    nc = _NC_CACHE["nc"]
    shared = {
        "w_in": np.ascontiguousarray(inp["w_in"], dtype=np.float32),
        "w_branch": np.ascontiguousarray(inp["w_branch"], dtype=np.float32),
        "w_out": np.ascontiguousarray(inp["w_out"], dtype=np.float32),
        "w_up": np.ascontiguousarray(inp["w_up"], dtype=np.float32),
        "w_down": np.ascontiguousarray(inp["w_down"], dtype=np.float32),
        "wgu": np.ascontiguousarray(inp["b_gate_up"], dtype=np.float32),
        "cols": cols, "lamv": lamv, "cst": cst,
    }
    in_maps = [dict(shared, x=np.ascontiguousarray(x[b])) for b in range(B)]
    res = run_bass_kernel_spmd(nc, in_maps, core_ids=list(range(B)))
    return np.stack([r["out"] for r in res.results], axis=0).astype(np.float32)
```

```python
import math
import numpy as np
import concourse.bass as bass
import concourse.mybir as mybir
from concourse.bass_utils import run_bass_kernel_spmd

F32 = mybir.dt.float32
BF16 = mybir.dt.bfloat16
AF = mybir.ActivationFunctionType
ALU = mybir.AluOpType

L_FULL, D, S_FULL, DIN, DFF = 4, 1024, 4096, 5136, 4096
TT = 512
EPS = 1e-6
NW = 3
NBLK = 33
NCOLS = 22
ENGS = ("pe", "act", "dve", "pool", "sp")


class Buf:
    __slots__ = ("name", "w", "r", "dsem", "dcnt")

    def __init__(self, name):
        self.name = name
        self.w = None
        self.r = {}
        self.dsem = None
        self.dcnt = 0


class Prog:
    def __init__(self, nc):
        self.nc = nc
        self.q = {e: [] for e in ENGS}
        self.sems = {}
        self.cnt = {e: 0 for e in ENGS}
        self.seen = {e: {} for e in ENGS}
        for e in ENGS:
            self.sems["E_" + e] = nc.alloc_semaphore(name="sem_" + e)
        self.ndsem = 0

    def _waits(self, eng, reads, writes):
        deps = {}
        for b in reads:
            if b.w is not None and deps.get(b.w[0], 0) < b.w[1]:
                deps[b.w[0]] = b.w[1]
        for b in writes:
            if b.w is not None and deps.get(b.w[0], 0) < b.w[1]:
                deps[b.w[0]] = b.w[1]
            for k, v in b.r.items():
                if deps.get(k, 0) < v:
                    deps[k] = v
        waits = []
        seen = self.seen[eng]
        for k, v in deps.items():
            if eng == "pe" and k == "E_pe":
                continue
            if seen.get(k, 0) >= v:
                continue
            seen[k] = v
            waits.append((k, v))
        return waits

    def _record(self, ev, reads, writes):
        k, v = ev
        for b in reads:
            if b.r.get(k, 0) < v:
                b.r[k] = v
        for b in writes:
            b.w = ev
            b.r = {}

    def op(self, eng, fn, reads=(), writes=()):
        waits = self._waits(eng, reads, writes)
        self.cnt[eng] += 1
        ev = ("E_" + eng, self.cnt[eng])
        self.q[eng].append((waits, fn, ("E_" + eng, 1)))
        self._record(ev, reads, writes)
        return ev

    def dma(self, eng, out, in_, home, reads=(), writes=()):
        if home.dsem is None:
            home.dsem = {}
            home.dcnt = {}
        if eng not in home.dsem:
            key = "D%d" % self.ndsem
            self.ndsem += 1
            self.sems[key] = self.nc.alloc_semaphore(name="dsem_%s" % key)
            home.dsem[eng] = key
            home.dcnt[eng] = 0
        waits = self._waits(eng, reads, writes)
        home.dcnt[eng] += 16
        ev = (home.dsem[eng], home.dcnt[eng])

        def fn(e, out=out, in_=in_):
            return e.dma_start(out=out, in_=in_)
        self.q[eng].append((waits, fn, (ev[0], 16)))
        self._record(ev, reads, writes)
        return ev

    def wait_all(self, eng, bufs):
        waits = self._waits(eng, bufs, ())
        self.q[eng].append((waits, None, None))

    def emit(self):
        nc = self.nc
        names = {"pe": "tensor", "act": "scalar", "dve": "vector", "pool": "gpsimd", "sp": "sync"}
        with nc.Block() as block:
            for e in ENGS:
                def body(eh, items=self.q[e], sems=self.sems):
                    for waits, fn, inc in items:
                        for k, v in waits:
                            eh.wait_ge(sems[k], v)
                        if fn is not None:
                            fn(eh).then_inc(sems[inc[0]], inc[1])
                getattr(block, names[e])(body)


def build(n_layers=L_FULL, n_tiles=S_FULL // TT, dbg_stop=False):
    S = n_tiles * TT
    nc = bass.Bass("TRN2", target_bir_lowering=False)
    x_in = nc.dram_tensor("x", [S, D], F32, kind="ExternalInput").ap()
    w_in = nc.dram_tensor("w_in", [L_FULL, D, DIN], F32, kind="ExternalInput").ap()
    w_br = nc.dram_tensor("w_branch", [L_FULL, 2, 512, D], F32, kind="ExternalInput").ap()
    w_out = nc.dram_tensor("w_out", [L_FULL, D, D], F32, kind="ExternalInput").ap()
    w_up = nc.dram_tensor("w_up", [L_FULL, D, DFF], F32, kind="ExternalInput").ap()
    w_dn = nc.dram_tensor("w_down", [L_FULL, DFF, D], F32, kind="ExternalInput").ap()
    wgu_in = nc.dram_tensor("wgu", [L_FULL, 16, 256], F32, kind="ExternalInput").ap()
    cols_in = nc.dram_tensor("cols", [128, L_FULL * NCOLS], F32, kind="ExternalInput").ap()
    lamv_in = nc.dram_tensor("lamv", [128, 4 * L_FULL * 64], F32, kind="ExternalInput").ap()
    cst_in = nc.dram_tensor("cst", [128, 1280], F32, kind="ExternalInput").ap()
    out = nc.dram_tensor("out", [S, D], F32, kind="ExternalOutput").ap()
    wsc = [nc.dram_tensor("wsc%d" % l, [NBLK, 128, 4096], BF16, kind="Internal").ap() for l in range(n_layers)]

    P = Prog(nc)
    def A(name, shape, dt):
        return nc.alloc_sbuf_tensor("s_" + name, shape, dt)
    kT = A("kT", [128, 4, S], BF16)
    vc = A("vc", [128, n_tiles * 4, 512], BF16)
    Sst = A("Sst", [128, 2, 128], F32)
    xs = A("xs", [128, 2, 1024], F32)
    xt = A("xt", [128, 4, 1024], F32)
    uT = A("uT", [128, 8, 512], BF16)
    wbuf = A("wbuf", [128, NW, 4096], BF16)
    qT = A("qT", [128, 8, 512], BF16)
    PT = A("PT", [128, 4, 512], BF16)
    nt = A("nt", [128, 4, 512], F32)
    sqb = A("sqb", [128, 4, 512], BF16)
    ev = A("ev", [128, 2, 512], F32)
    sacc = A("sacc", [128, 2, 512], F32)
    yT = A("yT", [128, 8, 512], BF16)
    Rt = A("Rt", [128, 16384], BF16)
    cstf = A("cstf", [128, 1280], F32)
    cstb = A("cstb", [128, 640], BF16)
    cols = A("cols", [128, L_FULL * NCOLS], F32)
    dcol = A("dcol", [128, L_FULL * 8], F32)
    lamv = A("lamv", [128, 4 * L_FULL * 64], F32)
    lamt = A("lamt", [128, 4 * L_FULL], F32)
    wgu = A("wgu", [16, L_FULL, 256], BF16)
    st = A("st", [128, 16], F32)
    dec = A("dec", [128, 2, 8], F32)
    dbgz = A("dbgz", [128, 4, 512], F32) if dbg_stop else None
    ps = [nc.alloc_psum_tensor("ps%d" % i, [128, 512], F32) for i in range(8)]

    actT = Rt[:, :].rearrange("p (f t) -> p f t", t=512)
    fv = [Rt[:, i * 1024:(i + 1) * 1024].bitcast(F32) for i in range(5)]
    qdT = Rt[:, 5120:6144].rearrange("p (h t) -> p h t", t=512)
    kdT = Rt[:, 6144:7168].rearrange("p (h t) -> p h t", t=512)
    ks_t = Rt[:, 7168:8192].rearrange("p (h j c) -> p h j c", h=2, j=4)
    bv_t = Rt[:, 8192:10240].rearrange("p (j c) -> p j c", c=512)
    Sbf = Rt[:, 10240:12288].rearrange("p (h n e) -> p h n e", h=2, n=8)
    baT = Rt[:, 12288:12800]
    aTm = [Rt[:, 12800 + 512 * i:13312 + 512 * i].rearrange("p (j c) -> p j c", c=128) for i in range(4)]

    def mk(n, k=None):
        return Buf(n) if k is None else [Buf("%s%d" % (n, i)) for i in range(k)]
    kTb, vcb = mk("kT", n_tiles), mk("vc", n_tiles)
    Sb, xsb, xtb, uTb, wb = mk("S", 2), mk("xs", 2), mk("xt", 4), mk("uT", 4), mk("wb", NW)
    qTb, PTb, ntb, sqbb, yTb = mk("qT", 8), mk("PT", 4), mk("nt", 4), mk("sqb", 4), mk("yT", 8)
    mTb = PTb + sqbb
    mT_ap = [PT[:, i, :] for i in range(4)] + [sqb[:, i, :] for i in range(4)]
    evb, saccb = mk("ev", 2), mk("sacc", 2)
    actb, fvb = mk("act", 32), mk("fv", 5)
    qdb, kdb, kstb, sbfb, aTb = mk("qd", 2), mk("kd", 2), mk("kst", 2), mk("sbf", 2), mk("aT", 4)
    bvb, bab, decb, stb, gR = mk("bv"), mk("ba"), mk("dec", 2), mk("st"), mk("gR")
    cstB, psb = mk("cst"), mk("ps", 8)
    xd = [mk("xd%d_" % t, 4) for t in range(n_tiles)]
    cvb = [[Buf("cv%d_%d" % (l, g)) for g in range(8)] for l in range(n_layers)]

    ident = cstf[:, 0:128]
    rmask = cstf[:, 128:640]
    ones_b = cstb[:, 0:128]
    ones_f = cstf[:, 640:768]
    bd64_b = cstb[:, 128:256]
    o128_b = cstb[:, 256:384]
    mask2_b = cstb[:, 384:512]

    def blk_group(b):
        return min(7, b * 8 // NBLK)

    def conv_layer_ops(l):
        W = wsc[l]
        ops = []

        def kview(src2d):
            return src2d.rearrange("(k p) c -> p k c", p=128)

        def dst(b, k, c, c0=0, cw=None):
            v = W[b, :, 0:k * c].rearrange("p (k c) -> p k c", c=c)
            return v if cw is None else v[:, :, c0:c0 + cw]
        wi = w_in[l]
        ops.append((0, [(dst(0, 8, 512), kview(wi[:, 0:512]))]))
        ops.append((1, [(dst(1, 8, 512), kview(wi[:, 512:1024]))]))
        ops.append((2, [(dst(2, 8, 16), kview(wi[:, 2560:2576]))]))
        ops.append((3, [(dst(3, 8, 512), kview(wi[:, 1024:1536]))]))
        ops.append((4, [(dst(4, 8, 512), kview(wi[:, 1536:2048]))]))
        ops.append((5, [(dst(5, 8, 512), kview(wi[:, 2048:2560]))]))
        ops.append((6, [(dst(6, 8, 512), kview(wi[:, 2576:3088]))]))
        for i in range(4):
            g0 = 3088 + 2 * i * 128
            ops.append((7 + 2 * i, [(dst(7 + 2 * i, 8, 512, 0, 256), kview(wi[:, g0:g0 + 256])),
                                    (dst(7 + 2 * i, 8, 512, 256, 256), kview(wi[:, g0 + 1024:g0 + 1280]))]))
            b = 8 + 2 * i
            pairs = []
            for n in range(2):
                d = W[b, :, n * 1024:(n + 1) * 1024].rearrange("p (k c) -> p k c", c=256)
                pairs.append((d, kview(w_br[l, n][:, 2 * i * 128:2 * i * 128 + 256])))
            ops.append((b, pairs))
        for h in range(2):
            ops.append((15 + h, [(dst(15 + h, 8, 512), kview(w_out[l][:, h * 512:(h + 1) * 512]))]))
        for i in range(8):
            ops.append((17 + i, [(dst(17 + i, 8, 512), kview(w_up[l][:, i * 512:(i + 1) * 512]))]))
        for h in range(2):
            for fg in range(4):
                b = 25 + h * 4 + fg
                ops.append((b, [(dst(b, 8, 512), kview(w_dn[l][fg * 1024:(fg + 1) * 1024, h * 512:(h + 1) * 512]))]))
        return ops

    def emit_conv(l, groups):
        for b, pairs in conv_layer_ops(l):
            g = blk_group(b)
            if g in groups:
                for d, s_ in pairs:
                    P.dma("pool", d, s_, cvb[l][g], writes=[cvb[l][g]])

    P.dma("sp", cstf[:], cst_in, cstB, writes=[cstB])
    P.dma("sp", cols[:], cols_in, cstB, writes=[cstB])
    P.dma("sp", lamv[:], lamv_in, cstB, writes=[cstB])
    P.dma("pool", wgu[:], wgu_in.rearrange("l r c -> r l c"), cstB, writes=[cstB])
    emit_conv(0, set(range(8)))
    P.op("dve", lambda e: e.tensor_copy(out=cstb[:], in_=cstf[:, 640:1280]), reads=[cstB], writes=[cstB])
    P.op("dve", lambda e: e.memset(qT[:, :, :], 0.0), reads=[], writes=qTb)
    lv = lamv[:, :].rearrange("p (a l d) -> p a l d", a=4, l=L_FULL)
    P.op("dve", lambda e: e.tensor_tensor(out=lv[:, 0], in0=lv[:, 0], in1=lv[:, 1], op=ALU.mult),
         reads=[cstB], writes=[cstB])
    P.op("dve", lambda e: e.tensor_tensor(out=lv[:, 2], in0=lv[:, 2], in1=lv[:, 3], op=ALU.mult),
         reads=[cstB], writes=[cstB])
    P.op("dve", lambda e: e.tensor_reduce(out=lamt[:, 0:L_FULL], in_=lv[:, 0], axis=mybir.AxisListType.X,
                                          op=ALU.add), reads=[cstB], writes=[cstB])
    P.op("dve", lambda e: e.tensor_reduce(out=lamt[:, L_FULL:2 * L_FULL], in_=lv[:, 2], axis=mybir.AxisListType.X,
                                          op=ALU.add), reads=[cstB], writes=[cstB])
    P.op("act", lambda e: e.activation(out=lamt[:, 2 * L_FULL:4 * L_FULL], in_=lamt[:, 0:2 * L_FULL], func=AF.Exp),
         reads=[cstB], writes=[cstB])
    P.op("dve", lambda e: e.tensor_tensor(out=lamt[:, 0:L_FULL], in0=lamt[:, 3 * L_FULL:4 * L_FULL],
                                          in1=lamt[:, 2 * L_FULL:3 * L_FULL], op=ALU.subtract),
         reads=[cstB], writes=[cstB])
    for l in range(n_layers):
        li = 0.8 - 0.6 * math.exp(-0.3 * l)
        c = l * NCOLS
        dc_ = l * 8
        P.op("dve", lambda e, l=l, li=li, dc_=dc_: e.tensor_scalar(
            out=dcol[:, dc_:dc_ + 1], in0=lamt[:, l:l + 1], scalar1=-li, scalar2=None, op0=ALU.add),
            reads=[cstB], writes=[cstB])
        P.op("dve", lambda e, c=c, dc_=dc_: e.tensor_scalar(
            out=dcol[:, dc_ + 1:dc_ + 2], in0=cols[:, c + 20:c + 21], scalar1=0.125, scalar2=None, op0=ALU.mult),
            reads=[cstB], writes=[cstB])
        P.op("dve", lambda e, c=c, dc_=dc_, li=li: e.tensor_scalar(
            out=dcol[:, dc_ + 2:dc_ + 3], in0=cols[:, c + 18:c + 19], scalar1=1.0 - li, scalar2=None, op0=ALU.mult),
            reads=[cstB], writes=[cstB])
        P.op("dve", lambda e, c=c, dc_=dc_: e.tensor_scalar(
            out=dcol[:, dc_ + 3:dc_ + 5], in0=cols[:, c + 16:c + 18], scalar1=-1.0, scalar2=None, op0=ALU.mult),
            reads=[cstB], writes=[cstB])

    state = {"wslot": 0, "gen": 0, "wide": 0, "xsl": 0, "stc": 0}

    def load_block(l, b, nelem=4096):
        s = state["wslot"]
        state["wslot"] = (s + 1) % NW
        P.dma("sp", wbuf[:, s, 0:nelem], wsc[l][b, :, 0:nelem], wb[s],
              reads=[cvb[l][blk_group(b)]], writes=[wb[s]])
        return s

    def genbank():
        i = state["gen"]
        state["gen"] = (i + 1) % 4
        return i

    def widebank():
        i = state["wide"]
        state["wide"] = (i + 1) % 6
        return 2 + i

    def w3(s, c=512, k=8):
        return wbuf[:, s, 0:k * c].rearrange("p (k c) -> p k c", c=c)

    def mm_group(outs, reads, writes):
        def fn(e):
            ins = None
            for o, pairs in outs:
                n = len(pairs)
                for i, (a, b) in enumerate(pairs):
                    ins = e.matmul(o, lhsT=a, rhs=b, start=(i == 0), stop=(i == n - 1))
            return ins
        return P.op("pe", fn, reads=reads, writes=writes)

    def rstd_from_ms(ms_ap, out_ap, reads, writes, tmpbuf, tmp_ap):
        P.op("act", lambda e: e.activation(out=tmp_ap, in_=ms_ap, func=AF.Ln, bias=EPS_AP),
             reads=reads + [cstB], writes=[tmpbuf])
        P.op("act", lambda e: e.activation(out=out_ap, in_=tmp_ap, func=AF.Exp, scale=-0.5),
             reads=[tmpbuf], writes=writes)

    epsc = dcol[:, L_FULL * 8 - 1:L_FULL * 8]
    P.op("dve", lambda e: e.memset(epsc, EPS), reads=[], writes=[cstB])
    EPS_AP = epsc

    def norm_T(l, tt, which, src_dram):
        for j in range(4):
            norm_sub(l, tt, which, src_dram, j)

    def norm_sub(l, tt, which, src_dram, j):
        norm_B(*norm_A(l, tt, which, src_dram, j))

    def norm_A(l, tt, which, src_dram, j):
        if True:
            sl = state["xsl"]
            state["xsl"] = 1 - sl
            sc = state["stc"]
            state["stc"] = (sc + 1) % 4
            c = sc * 4
            if which == 0:
                r0 = tt * TT + j * 128
                P.dma("pool", xs[:, sl, :], src_dram[r0:r0 + 128, :], xsb[sl],
                      reads=[xd[tt][j]], writes=[xsb[sl]])
                src, srcb = xs[:, sl, :], xsb[sl]
            else:
                src, srcb = xt[:, j, :], xtb[j]
            jk = ev[:, :, :].rearrange("p a b -> p (a b)")
            P.op("act", lambda e, src=src, c=c: e.activation(out=jk, in_=src, func=AF.Square,
                                                               accum_out=st[:, c:c + 1]),
                 reads=[srcb], writes=[evb[0], evb[1], stb])
            P.op("act", lambda e, c=c: e.activation(out=st[:, c + 1:c + 2], in_=st[:, c:c + 1], func=AF.Ln,
                                                    scale=1.0 / D, bias=EPS_AP), reads=[stb, cstB], writes=[stb])
            P.op("act", lambda e, c=c: e.activation(out=st[:, c + 2:c + 3], in_=st[:, c + 1:c + 2], func=AF.Exp,
                                                    scale=-0.5), reads=[stb], writes=[stb])
            P.op("dve", lambda e, src=src, sl=sl, c=c: e.tensor_scalar(
                out=xs[:, sl, :], in0=src, scalar1=st[:, c + 2:c + 3], scalar2=None, op0=ALU.mult),
                reads=[srcb, stb], writes=[xsb[sl]])
        return (l, which, j, sl)

    def norm_B(l, which, j, sl):
        gc0 = l * NCOLS + (0 if which == 0 else 8)
        if True:
            for half in range(2):
                bk = genbank()

                def tr(e, sl=sl, half=half, bk=bk):
                    ins = None
                    for k in range(4):
                        kc = half * 4 + k
                        ins = e.transpose(out=ps[bk][:, k * 128:(k + 1) * 128],
                                          in_=xs[:, sl, kc * 128:(kc + 1) * 128], identity=ident)
                    return ins
                P.op("pe", tr, reads=[xsb[sl], cstB], writes=[psb[bk]])
                g = cols[:, gc0 + half * 4:gc0 + half * 4 + 4].unsqueeze(2).to_broadcast([128, 4, 128])
                P.op("dve", lambda e, half=half, bk=bk, j=j, g=g: e.tensor_tensor(
                    out=uT[:, half * 4:half * 4 + 4, j * 128:(j + 1) * 128],
                    in0=ps[bk][:, :].rearrange("p (k t) -> p k t", t=128), in1=g, op=ALU.mult),
                    reads=[psb[bk], cstB], writes=[uTb[j]])

    def fm_chunk(s, c0, bk, extra_reads=()):
        w = w3(s)
        mm_group([(ps[bk][:, :], [(w[:, kc, c0:c0 + 128], uT[:, kc, :]) for kc in range(8)])],
                 reads=[wb[s]] + uTb + list(extra_reads), writes=[psb[bk]])

    def tile(l, tt):
        src_dram = x_in if l == 0 else out
        C = l * NCOLS
        DC = l * 8
        s = load_block(l, 2, 128)
        bk = genbank()
        wv = w3(s, 16, 8)
        mm_group([(ps[bk][0:16, :], [(wv[:, kc, :], uT[:, kc, :]) for kc in range(8)])],
                 reads=[wb[s]] + uTb, writes=[psb[bk]])
        P.op("dve", lambda e, bk=bk: e.tensor_copy(out=baT[0:16, :], in_=ps[bk][0:16, :]),
             reads=[psb[bk]], writes=[bab, gR])
        s = load_block(l, 3)
        w = w3(s)
        for j in range(4):
            bk = genbank()
            mm_group([(ps[bk][:, :], [(uT[:, kc, j * 128:(j + 1) * 128], w[:, kc, :]) for kc in range(8)])],
                     reads=[wb[s]] + uTb, writes=[psb[bk]])
            P.op("act", lambda e, bk=bk, j=j: e.activation(out=vc[:, tt * 4 + j, :], in_=ps[bk][:, :], func=AF.Copy),
                 reads=[psb[bk]], writes=[vcb[tt]])
        s_qk = load_block(l, 4)
        s_bv = load_block(l, 5)
        wbv = w3(s_bv)
        for hc in range(2):
            bk = genbank()
            mm_group([(ps[bk][:, :], [(wgu[:, l, hc * 128:(hc + 1) * 128], baT[0:16, :])])],
                     reads=[bab, cstB, gR], writes=[psb[bk]])
            nb = dcol[:, DC + 3 + hc:DC + 4 + hc]
            P.op("act", lambda e, bk=bk, nb=nb: e.activation(out=fv[0], in_=ps[bk][:, :], func=AF.Exp,
                                                             scale=-1.0, bias=nb),
                 reads=[psb[bk], cstB, gR], writes=[fvb[0]])
            P.op("act", lambda e: e.activation(out=fv[0], in_=fv[0], func=AF.Ln, bias=ONE_AP),
                 reads=[fvb[0], cstB, gR], writes=[fvb[0]])
            P.op("dve", lambda e: e.tensor_tensor_scan(out=fv[1], data0=rmask, data1=fv[0], initial=0.0,
                                                       op0=ALU.mult, op1=ALU.add),
                 reads=[fvb[0], cstB, gR], writes=[fvb[1]])
            P.op("act", lambda e: e.activation(out=fv[2], in_=fv[1], func=AF.Exp, scale=-1.0 / 16),
                 reads=[fvb[1], gR], writes=[fvb[2]])
            P.op("act", lambda e: e.activation(out=fv[0], in_=fv[1], func=AF.Exp, scale=1.0 / 16),
                 reads=[fvb[1], gR], writes=[fvb[0]])
            csv = fv[1].rearrange("p (n t) -> p n t", t=64)
            P.op("dve", lambda e, csv=csv: e.tensor_tensor(
                out=fv[3].rearrange("p (n t) -> p n t", t=64), in0=csv[:, :, 63:64].to_broadcast([128, 8, 64]),
                in1=csv, op=ALU.subtract), reads=[fvb[1], gR], writes=[fvb[3]])
            P.op("act", lambda e: e.activation(out=fv[3], in_=fv[3], func=AF.Exp, scale=-1.0 / 16),
                 reads=[fvb[3], gR], writes=[fvb[3]])
            egv = fv[2].rearrange("p (n t) -> p n t", t=64)
            P.op("dve", lambda e, hc=hc, egv=egv: e.tensor_copy(out=dec[:, hc, :], in_=egv[:, :, 63]),
                 reads=[fvb[2], gR], writes=[decb[hc]])
            for j in (2 * hc, 2 * hc + 1):
                bkv = genbank()
                mm_group([(ps[bkv][:, :], [(uT[:, kc, j * 128:(j + 1) * 128], wbv[:, kc, :]) for kc in range(8)])],
                         reads=[wb[s_bv]] + uTb, writes=[psb[bkv]])
                P.op("act", lambda e, bkv=bkv, j=j: e.activation(out=bv_t[:, j, :], in_=ps[bkv][:, :], func=AF.Copy),
                     reads=[psb[bkv], gR], writes=[bvb])
            bq = genbank()
            fm_chunk(s_qk, hc * 128, bq)
            P.op("dve", lambda e, bq=bq, hc=hc: e.scalar_tensor_tensor(
                out=qdT[:, hc, :], in0=ps[bq][:, :], scalar=0.125, in1=fv[2], op0=ALU.mult, op1=ALU.mult),
                reads=[psb[bq], fvb[2], gR], writes=[qdb[hc]])
            bkk = genbank()
            fm_chunk(s_qk, 256 + hc * 128, bkk)
            P.op("dve", lambda e, bkk=bkk, hc=hc: e.tensor_tensor(
                out=kdT[:, hc, :], in0=ps[bkk][:, :], in1=fv[0], op=ALU.mult),
                reads=[psb[bkk], fvb[0], gR], writes=[kdb[hc]])
            P.op("dve", lambda e, bkk=bkk: e.tensor_tensor(
                out=fv[4], in0=ps[bkk][:, :], in1=fv[3], op=ALU.mult),
                reads=[psb[bkk], fvb[3], gR], writes=[fvb[4]])
            bt = genbank()

            def trk(e, bt=bt):
                ins = None
                for j in range(4):
                    ins = e.transpose(out=ps[bt][:, j * 128:(j + 1) * 128], in_=fv[4][:, j * 128:(j + 1) * 128],
                                      identity=ident)
                return ins
            P.op("pe", trk, reads=[fvb[4], cstB, gR], writes=[psb[bt]])
            P.op("act", lambda e, bt=bt, hc=hc: e.activation(
                out=ks_t[:, hc, :, :], in_=ps[bt][:, :].rearrange("p (j c) -> p j c", c=128), func=AF.Copy),
                reads=[psb[bt], gR], writes=[kstb[hc]])
        for h in range(4):
            hc, hh = h // 2, h % 2
            pr = slice(hh * 64, (hh + 1) * 64)
            ba_ = genbank()
            mm_group([(ps[ba_][:, j * 128:(j + 1) * 128],
                       [(kdT[pr, hc, j * 128:(j + 1) * 128], qdT[pr, hc, j * 128:(j + 1) * 128])]) for j in range(4)],
                     reads=[kdb[hc], qdb[hc], gR], writes=[psb[ba_]])
            P.op("dve", lambda e, ba_=ba_, h=h: e.tensor_tensor(
                out=aTm[h], in0=ps[ba_][:, :].rearrange("p (j c) -> p j c", c=128),
                in1=mask2_b.unsqueeze(1).to_broadcast([128, 4, 128]), op=ALU.mult),
                reads=[psb[ba_], cstB, gR], writes=[aTb[h]])
        if tt == 0:
            P.op("dve", lambda e: e.memset(Sst[:, :, :], 0.0), reads=[], writes=[Sb[0], Sb[1]])

        def s_step(n):
            j, half = n // 2, n % 2
            kb = [0, 1]
            if half == 0:
                for hf in range(2):
                    pr = slice(hf * 64, (hf + 1) * 64)
                    outs = []
                    for h in range(4):
                        outs.append((ps[kb[hf]][:, h * 128:(h + 1) * 128],
                                     [(ks_t[pr, h // 2, j, :], bv_t[pr, j, h * 128:(h + 1) * 128])]))
                    mm_group(outs, reads=[kstb[0], kstb[1], bvb, gR], writes=[psb[kb[hf]]])
            for hc in range(2):
                P.op("act", lambda e, hc=hc: e.activation(out=Sbf[:, hc, n, :], in_=Sst[:, hc, :], func=AF.Copy),
                     reads=[Sb[hc], gR], writes=[sbfb[hc]])
                for hh in range(2):
                    h = 2 * hc + hh
                    pr = slice(hh * 64, (hh + 1) * 64)
                    P.op("dve", lambda e, hc=hc, h=h, pr=pr: e.scalar_tensor_tensor(
                        out=Sst[pr, hc, :], in0=Sst[pr, hc, :], scalar=dec[pr, hc, n:n + 1],
                        in1=ps[kb[half]][pr, h * 128:(h + 1) * 128], op0=ALU.mult, op1=ALU.add),
                        reads=[Sb[hc], decb[hc], psb[kb[half]]], writes=[Sb[hc]])
        sblk = [load_block(l, 0), load_block(l, 1)]
        prev = None

        def qk_tail(c, bk):
            isk, h = c // 4, c % 4
            gcol = dcol[:, DC + 1:DC + 2] if isk == 0 else cols[:, C + 21:C + 22]
            sq = sqb[:, c % 2, :]
            b2 = widebank()
            mm_group([(ps[b2][:, :], [(bd64_b, sq)])], reads=[sqbb[c % 2], cstB], writes=[psb[b2]])
            ti = c % 2
            rstd_from_ms(ps[b2][:, :], nt[:, ti, :], [psb[b2]], [ntb[ti]], ntb[2 + ti], nt[:, 2 + ti, :])
            if isk == 0:
                for m in range(2):
                    pr = slice(m * 64, (m + 1) * 64)
                    P.op("dve", lambda e, m=m, pr=pr: e.scalar_tensor_tensor(
                        out=qT[pr, 2 * h + m, :], in0=ps[bk][pr, :], scalar=gcol[pr, :], in1=nt[pr, ti, :],
                        op0=ALU.mult, op1=ALU.mult),
                        reads=[psb[bk], ntb[ti], cstB], writes=[qTb[2 * h + m]])
            else:
                o_ap, o_b = kT[:, h, tt * TT:(tt + 1) * TT], [kTb[tt]]
                P.op("dve", lambda e: e.scalar_tensor_tensor(
                    out=o_ap, in0=ps[bk][:, :], scalar=gcol, in1=nt[:, ti, :], op0=ALU.mult, op1=ALU.mult),
                    reads=[psb[bk], ntb[ti], cstB], writes=o_b)
        for c in range(8):
            bk = widebank()
            fm_chunk(sblk[c // 4], (c % 4) * 128, bk)
            sq = sqb[:, c % 2, :]
            P.op("act", lambda e, bk=bk, sq=sq: e.activation(out=sq, in_=ps[bk][:, :], func=AF.Square),
                 reads=[psb[bk]], writes=[sqbb[c % 2]])
            s_step(c)
            if prev is not None:
                qk_tail(*prev)
            prev = (c, bk)
        qk_tail(*prev)
        nk = 4 * (tt + 1)

        def attn_main(h):
            items = [(m, kt) for kt in range(nk) for m in range(2)]
            pend = []

            def score(m, kt):
                bk = genbank()
                a = kt - 4 * tt
                c0 = a * 128 if a > 0 else 0
                mm_group([(ps[bk][:, c0:512], [(kT[:, h, kt * 128:(kt + 1) * 128], qT[:, 2 * h + m, c0:512])])],
                         reads=[kTb[kt // 4], qTb[2 * h + m]], writes=[psb[bk]])
                sl = bk
                P.op("act", lambda e: e.activation(out=PT[:, sl, c0:512], in_=ps[bk][:, c0:512], func=AF.Exp),
                     reads=[psb[bk]], writes=[PTb[sl]])
                if a >= 0:
                    P.op("dve", lambda e: e.memset(PT[64:128, sl, c0:c0 + 64], 0.0), reads=[], writes=[PTb[sl]])
                return (m, kt, sl, c0)

            def pv(m, kt, sl, c0):
                first, last = (kt == 0), (kt == nk - 1)
                o1 = ps[4 + m][:, c0:512]
                vv, pp = vc[:, kt, h * 128:(h + 1) * 128], PT[:, sl, c0:512]
                P.op("pe", lambda e: e.matmul(o1, lhsT=vv, rhs=pp, start=first, stop=last),
                     reads=[vcb[kt // 4], PTb[sl]], writes=[psb[4 + m]])
                if first:
                    P.op("dve", lambda e: e.tensor_copy(out=sacc[:, m, :], in_=pp), reads=[PTb[sl]], writes=[saccb[m]])
                else:
                    P.op("dve", lambda e: e.tensor_tensor(out=sacc[:, m, c0:512], in0=sacc[:, m, c0:512], in1=pp,
                                                          op=ALU.add), reads=[PTb[sl], saccb[m]], writes=[saccb[m]])
            for it in items:
                pend.append(score(*it))
                if len(pend) > 2:
                    pv(*pend.pop(0))
            while pend:
                pv(*pend.pop(0))

        def attn_evac(h):
            for m in range(2):
                mm_group([(ps[6 + m][:, :], [(ones_f, sacc[:, m, :])])], reads=[saccb[m], cstB], writes=[psb[6 + m]])
            P.op("dve", lambda e: e.tensor_copy(out=ev[:, 0, :], in_=ps[4][:, :]), reads=[psb[4]], writes=[evb[0]])
            P.op("dve", lambda e: e.tensor_copy(out=ev[:, 1, :], in_=ps[5][:, :]), reads=[psb[5]], writes=[evb[1]])
            for m in range(2):
                P.op("act", lambda e, m=m: e.activation(out=nt[:, 1 + 2 * m, :], in_=ps[6 + m][:, :], func=AF.Ln),
                     reads=[psb[6 + m]], writes=[ntb[1 + 2 * m]])
                P.op("act", lambda e, m=m: e.activation(out=nt[:, 1 + 2 * m, :], in_=nt[:, 1 + 2 * m, :], func=AF.Exp,
                                                        scale=-1.0), reads=[ntb[1 + 2 * m]], writes=[ntb[1 + 2 * m]])
                P.op("dve", lambda e, m=m: e.tensor_tensor(out=ev[:, m, :], in0=ev[:, m, :], in1=nt[:, 1 + 2 * m, :],
                                                           op=ALU.mult), reads=[evb[m], ntb[1 + 2 * m]], writes=[evb[m]])
            P.op("dve", lambda e: e.scalar_tensor_tensor(out=ev[:, 0, :], in0=ev[:, 1, :], scalar=dcol[:, DC:DC + 1],
                                                         in1=ev[:, 0, :], op0=ALU.mult, op1=ALU.add),
                 reads=[evb[1], evb[0], cstB], writes=[evb[0]])
            P.op("act", lambda e: e.activation(out=sqb[:, 2, :], in_=ev[:, 0, :], func=AF.Square),
                 reads=[evb[0]], writes=[sqbb[2]])

        def attn_tail(h):
            b2 = genbank()
            mm_group([(ps[b2][:, :], [(o128_b, sqb[:, 2, :])])], reads=[sqbb[2], cstB], writes=[psb[b2]])
            rstd_from_ms(ps[b2][:, :], nt[:, 0, :], [psb[b2]], [ntb[0]], ntb[2], nt[:, 2, :])
            P.op("dve", lambda e: e.scalar_tensor_tensor(
                out=yT[:, h, :], in0=ev[:, 0, :], scalar=dcol[:, DC + 2:DC + 3], in1=nt[:, 0, :],
                op0=ALU.mult, op1=ALU.mult), reads=[evb[0], ntb[0], cstB], writes=[yTb[h]])
        for h in range(4):
            attn_main(h)
            if h > 0:
                attn_tail(h - 1)
            attn_evac(h)
        s_br = load_block(l, 6)
        for h in range(4):
            hc, hh = h // 2, h % 2
            pr = slice(hh * 64, (hh + 1) * 64)
            ob = 4 + h

            def ofn(e, h=h, hc=hc, pr=pr, ob=ob):
                ins = None
                for j in range(4):
                    e.matmul(ps[ob][:, j * 128:(j + 1) * 128], lhsT=bv_t[:, j, h * 128:(h + 1) * 128],
                             rhs=aTm[h][:, j, :], start=True, stop=False)
                    for half in range(2):
                        n = 2 * j + half
                        ins = e.matmul(ps[ob][:, n * 64:(n + 1) * 64], lhsT=Sbf[pr, hc, n, :],
                                       rhs=qdT[pr, hc, n * 64:(n + 1) * 64], start=False, stop=(half == 1))
                return ins
            P.op("pe", ofn, reads=[bvb, aTb[h], sbfb[hc], qdb[hc], gR], writes=[psb[ob]])
            if h == 0:
                attn_tail(3)
            P.op("act", lambda e, ob=ob, h=h: e.activation(out=sqb[:, h, :], in_=ps[ob][:, :], func=AF.Square),
                 reads=[psb[ob]], writes=[sqbb[h]])
        for hp in range(2):
            gb = {}
            for h in (2 * hp, 2 * hp + 1):
                b2 = genbank()
                mm_group([(ps[b2][:, :], [(o128_b, sqb[:, h, :])])], reads=[sqbb[h], cstB], writes=[psb[b2]])
                b3 = genbank()
                fm_chunk(s_br, h * 128, b3)
                gb[h] = (b2, b3)
            for h in (2 * hp, 2 * hp + 1):
                b2, b3 = gb[h]
                ob = 4 + h
                rstd_from_ms(ps[b2][:, :], nt[:, 0, :], [psb[b2]], [ntb[0]], ntb[2], nt[:, 2, :])
                P.op("act", lambda e, b3=b3: e.activation(out=nt[:, 3, :], in_=ps[b3][:, :], func=AF.Silu),
                     reads=[psb[b3]], writes=[ntb[3]])
                P.op("dve", lambda e, ob=ob: e.scalar_tensor_tensor(
                    out=nt[:, 1, :], in0=ps[ob][:, :], scalar=cols[:, C + 19:C + 20], in1=nt[:, 0, :],
                    op0=ALU.mult, op1=ALU.mult), reads=[psb[ob], ntb[0], cstB], writes=[ntb[1]])
                P.op("dve", lambda e, h=h: e.tensor_tensor(out=yT[:, 4 + h, :], in0=nt[:, 1, :], in1=nt[:, 3, :],
                                                           op=ALU.mult), reads=[ntb[1], ntb[3]], writes=[yTb[4 + h]])
        for j in range(4):
            r0 = tt * TT + j * 128
            P.dma("pool", xt[:, j, :], src_dram[r0:r0 + 128, :], xtb[j], reads=[xd[tt][j]], writes=[xtb[j]])
        for i in range(4):
            sg = load_block(l, 7 + 2 * i)
            sb_ = load_block(l, 8 + 2 * i, 2048)
            wbr = wbuf[:, sb_, 0:2048].rearrange("p (n k c) -> p n k c", n=2, k=4)
            for dd in range(2):
                dcn = 2 * i + dd
                gb = []
                for n in range(2):
                    bk = genbank()
                    fm_chunk(sg, n * 256 + dd * 128, bk)
                    P.op("act", lambda e, bk=bk, n=n: e.activation(out=nt[:, n, :], in_=ps[bk][:, :], func=AF.Sigmoid),
                         reads=[psb[bk]], writes=[ntb[n]])
                for n in range(2):
                    bk = genbank()
                    gb.append(bk)
                    mm_group([(ps[bk][:, :], [(wbr[:, n, kc, dd * 128:(dd + 1) * 128], yT[:, 4 * n + kc, :])
                                              for kc in range(4)])],
                             reads=[wb[sb_]] + yTb[4 * n:4 * n + 4], writes=[psb[bk]])
                P.op("dve", lambda e, gb=gb: e.tensor_tensor(out=nt[:, 2, :], in0=ps[gb[0]][:, :], in1=nt[:, 0, :],
                                                             op=ALU.mult), reads=[psb[gb[0]], ntb[0]], writes=[ntb[2]])
                P.op("dve", lambda e, gb=gb: e.tensor_tensor(out=nt[:, 3, :], in0=ps[gb[1]][:, :], in1=nt[:, 1, :],
                                                             op=ALU.mult), reads=[psb[gb[1]], ntb[1]], writes=[ntb[3]])
                P.op("dve", lambda e, dcn=dcn: e.tensor_tensor(out=mT_ap[dcn], in0=nt[:, 2, :], in1=nt[:, 3, :],
                                                               op=ALU.add), reads=[ntb[2], ntb[3]], writes=[mTb[dcn]])
        so = [load_block(l, 15), load_block(l, 16)]
        if dbg_stop:
            return

        def outp(j):
            for half in range(2):
                w = w3(so[half])
                bk = genbank()
                mm_group([(ps[bk][:, :], [(mT_ap[kc][:, j * 128:(j + 1) * 128], w[:, kc, :]) for kc in range(8)])],
                         reads=[wb[so[half]]] + mTb, writes=[psb[bk]])
                P.op("dve", lambda e, bk=bk, half=half: e.tensor_tensor(
                    out=xt[:, j, half * 512:(half + 1) * 512], in0=ps[bk][:, :],
                    in1=xt[:, j, half * 512:(half + 1) * 512], op=ALU.add),
                    reads=[psb[bk], xtb[j]], writes=[xtb[j]])
        outp(0)
        outp(1)
        norm_sub(l, tt, 1, None, 0)
        outp(2)
        norm_sub(l, tt, 1, None, 1)
        outp(3)
        norm_sub(l, tt, 1, None, 2)
        norm_sub(l, tt, 1, None, 3)
        for i in range(8):
            s = load_block(l, 17 + i)
            for f in range(4):
                fc = 4 * i + f
                bk = genbank()
                fm_chunk(s, f * 128, bk)
                ti = fc % 2
                P.op("act", lambda e, bk=bk, ti=ti: e.activation(out=nt[:, ti, :], in_=ps[bk][:, :], func=AF.Relu),
                     reads=[psb[bk]], writes=[ntb[ti]])
                wr = [actb[fc]] + ([gR] if fc == 0 else [])
                rd = [ntb[ti]] + ([] if fc == 0 else [gR])
                P.op("dve", lambda e, ti=ti, fc=fc: e.tensor_tensor(out=actT[:, fc, :], in0=nt[:, ti, :],
                                                                    in1=nt[:, ti, :], op=ALU.mult),
                     reads=rd, writes=wr)
        nxt = (l, tt + 1) if tt + 1 < n_tiles else ((l + 1, 0) if l + 1 < n_layers else None)
        nsrc = None if nxt is None else (x_in if nxt[0] == 0 else out)
        hA = []
        if nxt is not None:
            hA = [norm_A(nxt[0], nxt[1], 0, nsrc, 0), norm_A(nxt[0], nxt[1], 0, nsrc, 1)]
        for half in range(2):
            for fg in range(4):
                if nxt is not None and half == 0 and fg >= 1:
                    norm_B(*hA.pop(0))
                    if fg + 1 < 4:
                        hA.append(norm_A(nxt[0], nxt[1], 0, nsrc, fg + 1))
                if nxt is not None and half == 1 and fg == 0:
                    norm_B(*hA.pop(0))
                s = load_block(l, 25 + half * 4 + fg)
                w = w3(s)

                def dfn(e, fg=fg, w=w):
                    ins = None
                    for j in range(4):
                        for f in range(8):
                            fc = fg * 8 + f
                            ins = e.matmul(ps[4 + j][:, :], lhsT=actT[:, fc, j * 128:(j + 1) * 128], rhs=w[:, f, :],
                                           start=(fc == 0), stop=(fc == 31))
                    return ins
                P.op("pe", dfn, reads=[wb[s], gR] + actb[fg * 8:fg * 8 + 8], writes=psb[4:8])
            for j in range(4):
                P.op("dve", lambda e, j=j, half=half: e.tensor_tensor(
                    out=xt[:, j, half * 512:(half + 1) * 512], in0=ps[4 + j][:, :],
                    in1=xt[:, j, half * 512:(half + 1) * 512], op=ALU.add),
                    reads=[psb[4 + j], xtb[j]], writes=[xtb[j]])
        for j in range(4):
            r0 = tt * TT + j * 128
            P.dma("pool", out[r0:r0 + 128, :], xt[:, j, :], xtb[j], reads=[xtb[j]], writes=[xd[tt][j]])
        if l + 1 < n_layers:
            emit_conv(l + 1, {tt} if n_tiles == 8 else set(range(8)) if tt == 0 else set())

    onec = dcol[:, L_FULL * 8 - 2:L_FULL * 8 - 1]
    P.op("dve", lambda e: e.memset(onec, 1.0), reads=[], writes=[cstB])
    ONE_AP = onec

    norm_T(0, 0, 0, x_in)
    for l in range(n_layers):
        for tt in range(n_tiles):
            tile(l, tt)
    P.wait_all("pool", [b for t in xd for b in t])
    P.emit()
    return nc


def host_consts():
    c = np.zeros((128, 1280), np.float32)
    c[:, 0:128] = np.eye(128, dtype=np.float32)
    rm = np.ones((128, 512), np.float32)
    rm[:, ::64] = 0.0
    c[:, 128:640] = rm
    c[:, 640:768] = 1.0
    bd = np.zeros((128, 128), np.float32)
    bd[:64, :64] = 1.0 / 64
    bd[64:, 64:] = 1.0 / 64
    c[:, 768:896] = bd
    c[:, 896:1024] = 1.0 / 128
    jj, ii = np.meshgrid(np.arange(128), np.arange(128), indexing="ij")
    m2 = ((ii >= jj) & ((ii // 64) == (jj // 64))).astype(np.float32)
    c[:, 1024:1152] = m2
    return c


def layout_params(inp, n_layers=L_FULL):
    cols = np.zeros((128, L_FULL * NCOLS), np.float32)
    for l in range(L_FULL):
        c = l * NCOLS
        cols[:, c:c + 8] = inp["norm_mix"][l].reshape(8, 128).T
        cols[:, c + 8:c + 16] = inp["norm_ffn"][l].reshape(8, 128).T
        cols[:, c + 16:c + 18] = inp["b_gate_bias"][l].reshape(2, 128).T
        cols[:, c + 18] = inp["a_sub_norm"][l]
        cols[:, c + 19] = inp["b_out_norm"][l]
        cols[:, c + 20] = np.concatenate([inp["a_q_norm"][l], inp["a_q_norm"][l]])
        cols[:, c + 21] = np.concatenate([inp["a_k_norm"][l], inp["a_k_norm"][l]])
    lam = np.stack([inp["a_lambda_q1"], inp["a_lambda_k1"], inp["a_lambda_q2"], inp["a_lambda_k2"]], 0)
    lamv = np.ascontiguousarray(np.broadcast_to(lam.reshape(1, -1), (128, 4 * L_FULL * 64))).astype(np.float32)
    return cols, lamv


_NC_CACHE = {}


def kernel(**inputs):
    inp = {k: np.asarray(v) for k, v in inputs.items()}
    x = np.ascontiguousarray(inp["x"], dtype=np.float32)
    B = x.shape[0]
    cols, lamv = layout_params(inp)
    cst = host_consts()
    if "nc" not in _NC_CACHE:
        _NC_CACHE["nc"] = build()
    nc = _NC_CACHE["nc"]
    shared = {
        "w_in": np.ascontiguousarray(inp["w_in"], dtype=np.float32),
        "w_branch": np.ascontiguousarray(inp["w_branch"], dtype=np.float32),
        "w_out": np.ascontiguousarray(inp["w_out"], dtype=np.float32),
        "w_up": np.ascontiguousarray(inp["w_up"], dtype=np.float32),
        "w_down": np.ascontiguousarray(inp["w_down"], dtype=np.float32),
        "wgu": np.ascontiguousarray(inp["b_gate_up"], dtype=np.float32),
        "cols": cols, "lamv": lamv, "cst": cst,
    }
    in_maps = [dict(shared, x=np.ascontiguousarray(x[b])) for b in range(B)]
    res = run_bass_kernel_spmd(nc, in_maps, core_ids=list(range(B)))
    return np.stack([r["out"] for r in res.results], axis=0).astype(np.float32)
```
